# Optimizing a Trainium2 kernel written in Bass

```python
import jax, jax.numpy as jnp
from jax import lax
import numpy as np

D_MODEL = 1024
BATCH = 2
SEQ = 8192
DEPTH = 1

CHUNK = 64
Q_BLOCK = 128
ROPE_THETA = 500000.0
EPS = 1e-6
NEG = -1e30

MLA_HEADS = 8
MLA_Q_LORA = 384
MLA_KV_LORA = 256
MLA_NOPE = 64
MLA_ROPE = 32
MLA_V = 64

DSA_HEADS = 8
DSA_HEAD_DIM = 64
DSA_ROT = DSA_HEAD_DIM // 4
IDX_HEADS = 8
IDX_DIM = 64
IDX_ROT = IDX_DIM // 4
DSA_TOPK_MAX = 256

N_GROUPS = 8
EXPERTS_PER_GROUP = 4
N_EXPERTS = N_GROUPS * EXPERTS_PER_GROUP
EXPERT_FF = 256
TOPK_IN_GROUP = 2

IN_SIZES = (
    MLA_Q_LORA,
    MLA_KV_LORA,
    MLA_ROPE,
    DSA_HEADS * DSA_HEAD_DIM,
    DSA_HEADS * DSA_HEAD_DIM,
    DSA_HEADS * DSA_HEAD_DIM,
    IDX_HEADS * IDX_DIM,
    IDX_DIM,
    IDX_HEADS,
    D_MODEL,
    D_MODEL,
)
IN_WIDTH = sum(IN_SIZES)

kernel_name = 'hybrid_mla_dsa_hier_moe_block'


def rmsnorm(x, g):
    xf = x.astype(jnp.float32)
    y = xf * lax.rsqrt(jnp.mean(xf * xf, axis=-1, keepdims=True) + EPS)
    return (y * g.astype(jnp.float32)).astype(x.dtype)


def rope(x, pos, rot_dim):
    half = rot_dim // 2
    inv_freq = ROPE_THETA ** (-jnp.arange(half, dtype=jnp.float32) * 2.0 / rot_dim)
    ang = pos.astype(jnp.float32)[:, None] * inv_freq[None, :]
    cos = jnp.cos(ang)[:, None, :]
    sin = jnp.sin(ang)[:, None, :]
    xr = x[..., :rot_dim].astype(jnp.float32)
    x1, x2 = xr[..., :half], xr[..., half:]
    rot = jnp.concatenate([x1 * cos - x2 * sin, x2 * cos + x1 * sin], axis=-1).astype(x.dtype)
    return jnp.concatenate([rot, x[..., rot_dim:]], axis=-1)


def _to_blocks(a):
    b, s = a.shape[0], a.shape[1]
    return a.reshape(b, s // Q_BLOCK, Q_BLOCK, *a.shape[2:]).swapaxes(0, 1)


def _from_blocks(a):
    a = a.swapaxes(0, 1)
    return a.reshape(a.shape[0], a.shape[1] * a.shape[2], *a.shape[3:])


def chunk_causal_attention(q, k, v, scale):
    s_len = q.shape[1]
    nb = s_len // Q_BLOCK
    key_chunk = jnp.arange(s_len) // CHUNK

    def one_block(args):
        qi, bi = args
        q_chunk = (bi * Q_BLOCK + jnp.arange(Q_BLOCK)) // CHUNK
        mask = key_chunk[None, :] <= q_chunk[:, None]
        s = jnp.einsum('bqhd,bkhd->bhqk', qi, k).astype(jnp.float32) * scale
        s = jnp.where(mask[None, None], s, NEG)
        p = jax.nn.softmax(s, axis=-1).astype(v.dtype)
        return jnp.einsum('bhqk,bkhd->bqhd', p, v)

    out = lax.map(one_block, (_to_blocks(q), jnp.arange(nb)))
    return _from_blocks(out)


def indexed_sparse_attention(q, k, v, q_idx, k_idx, w_idx, top_k):
    s_len = q.shape[1]
    nb = s_len // Q_BLOCK
    key_chunk = jnp.arange(s_len) // CHUNK
    gather = jax.vmap(lambda a, i: a[i])

    def one_block(args):
        qi, qii, wi, bi = args
        q_chunk = (bi * Q_BLOCK + jnp.arange(Q_BLOCK)) // CHUNK
        adm = key_chunk[None, :] <= q_chunk[:, None]
        logits = jnp.einsum('bqhd,bkd->bqhk', qii, k_idx).astype(jnp.float32) * (IDX_DIM ** -0.5)
        score = jnp.einsum('bqh,bqhk->bqk', wi.astype(jnp.float32), jax.nn.relu(logits))
        score = jnp.where(adm[None], score, -jnp.inf)
        _, sel = lax.top_k(score, top_k)
        sel_ok = (sel // CHUNK) <= q_chunk[None, :, None]
        k_sel = gather(k, sel)
        v_sel = gather(v, sel)
        s = jnp.einsum('bqhd,bqkhd->bhqk', qi, k_sel).astype(jnp.float32) * (DSA_HEAD_DIM ** -0.5)
        s = jnp.where(sel_ok[:, None], s, NEG)
        p = jax.nn.softmax(s, axis=-1).astype(v.dtype)
        return jnp.einsum('bhqk,bqkhd->bqhd', p, v_sel)

    out = lax.map(one_block, (_to_blocks(q), _to_blocks(q_idx), _to_blocks(w_idx), jnp.arange(nb)))
    return _from_blocks(out)


def hierarchical_moe(h, w_router_group, b_router_group, w_router_expert, b_router_expert,
                     w_gate, w_up, w_down):
    t = h.shape[0]
    group_logits = jnp.matmul(h, w_router_group).astype(jnp.float32) + b_router_group.astype(jnp.float32)
    p_group = jax.nn.softmax(group_logits, axis=-1)
    g_sel = jnp.argmax(group_logits, axis=-1)
    w_grp = jnp.take_along_axis(p_group, g_sel[:, None], axis=-1)
    exp_logits = (jnp.matmul(h, w_router_expert).astype(jnp.float32)
                  + b_router_expert.astype(jnp.float32)).reshape(t, N_GROUPS, EXPERTS_PER_GROUP)
    in_group = jnp.take_along_axis(exp_logits, g_sel[:, None, None], axis=1)[:, 0]
    top_vals, top_idx = lax.top_k(in_group, TOPK_IN_GROUP)
    w_top = jax.nn.softmax(top_vals, axis=-1) * w_grp
    expert_id = g_sel[:, None] * EXPERTS_PER_GROUP + top_idx
    combine = jnp.sum(jax.nn.one_hot(expert_id, N_EXPERTS, dtype=jnp.float32) * w_top[..., None],
                      axis=1).astype(h.dtype)
    out = jnp.zeros_like(h)
    for e in range(N_EXPERTS):
        hid = jax.nn.silu(jnp.matmul(h, w_gate[e])) * jnp.matmul(h, w_up[e])
        out = out + jnp.matmul(hid, w_down[e]) * combine[:, e:e + 1]
    return out


def hybrid_layer(x, pos, norm_mix_g, w_in, mla_q_norm_g, w_uq, mla_kv_norm_g, w_uk, w_uv,
                 w_o_a, w_o_b, w_out, norm_ffn_g, w_router_group, b_router_group,
                 w_router_expert, b_router_expert, w_gate, w_up, w_down):
    b, s, d = x.shape
    h = rmsnorm(x, norm_mix_g)
    proj = jnp.einsum('bsd,de->bse', h, w_in)
    splits = np.cumsum(IN_SIZES)[:-1].tolist()
    (c_q, c_kv, k_r, q_b, k_b, v_b, q_i, k_i, w_i, g_a, g_b) = jnp.split(proj, splits, axis=-1)

    c_q = rmsnorm(c_q, mla_q_norm_g)
    q_a = jnp.einsum('bsr,re->bse', c_q, w_uq).reshape(b, s, MLA_HEADS, MLA_NOPE + MLA_ROPE)
    q_a = jnp.concatenate([q_a[..., :MLA_NOPE], rope(q_a[..., MLA_NOPE:], pos, MLA_ROPE)], axis=-1)
    c_kv = rmsnorm(c_kv, mla_kv_norm_g)
    k_nope = jnp.einsum('bsr,re->bse', c_kv, w_uk).reshape(b, s, MLA_HEADS, MLA_NOPE)
    v_a = jnp.einsum('bsr,re->bse', c_kv, w_uv).reshape(b, s, MLA_HEADS, MLA_V)
    k_pe = rope(k_r[:, :, None, :], pos, MLA_ROPE)
    k_a = jnp.concatenate([k_nope, jnp.broadcast_to(k_pe, (b, s, MLA_HEADS, MLA_ROPE))], axis=-1)
    o_a = chunk_causal_attention(q_a, k_a, v_a, (MLA_NOPE + MLA_ROPE) ** -0.5)
    y_a = jnp.einsum('bse,ed->bsd', o_a.reshape(b, s, MLA_HEADS * MLA_V), w_o_a)

    q_b = rope(q_b.reshape(b, s, DSA_HEADS, DSA_HEAD_DIM), pos, DSA_ROT)
    k_b = rope(k_b.reshape(b, s, DSA_HEADS, DSA_HEAD_DIM), pos, DSA_ROT)
    v_b = v_b.reshape(b, s, DSA_HEADS, DSA_HEAD_DIM)
    q_i = rope(q_i.reshape(b, s, IDX_HEADS, IDX_DIM), pos, IDX_ROT)
    k_i = rope(k_i[:, :, None, :], pos, IDX_ROT)[:, :, 0, :]
    top_k = min(DSA_TOPK_MAX, s // 4)
    o_b = indexed_sparse_attention(q_b, k_b, v_b, q_i, k_i, w_i * (IDX_HEADS ** -0.5), top_k)
    y_b = jnp.einsum('bse,ed->bsd', o_b.reshape(b, s, DSA_HEADS * DSA_HEAD_DIM), w_o_b)

    y = jax.nn.sigmoid(g_a) * y_a + jax.nn.sigmoid(g_b) * y_b
    x = x + jnp.einsum('bsd,de->bse', y, w_out)

    h2 = rmsnorm(x, norm_ffn_g).reshape(b * s, d)
    m = hierarchical_moe(h2, w_router_group, b_router_group, w_router_expert, b_router_expert,
                         w_gate, w_up, w_down)
    return x + m.reshape(b, s, d)


def setup_inputs(seed: int = 0) -> dict:
    key = jax.random.key(seed)
    ks = jax.random.split(key, 20)
    f32 = jnp.float32
    L = DEPTH

    def dense(k, shape, fan_in):
        return jax.random.normal(k, shape, f32) * (fan_in ** -0.5)

    def gain(k, shape):
        return 1.0 + 0.05 * jax.random.normal(k, shape, f32)

    return {
        'x': jax.random.normal(ks[0], (BATCH, SEQ, D_MODEL), f32),
        'norm_mix_g': gain(ks[1], (L, D_MODEL)),
        'w_in': dense(ks[2], (L, D_MODEL, IN_WIDTH), D_MODEL),
        'mla_q_norm_g': gain(ks[3], (L, MLA_Q_LORA)),
        'w_uq': dense(ks[4], (L, MLA_Q_LORA, MLA_HEADS * (MLA_NOPE + MLA_ROPE)), MLA_Q_LORA),
        'mla_kv_norm_g': gain(ks[5], (L, MLA_KV_LORA)),
        'w_uk': dense(ks[6], (L, MLA_KV_LORA, MLA_HEADS * MLA_NOPE), MLA_KV_LORA),
        'w_uv': dense(ks[7], (L, MLA_KV_LORA, MLA_HEADS * MLA_V), MLA_KV_LORA),
        'w_o_a': dense(ks[8], (L, MLA_HEADS * MLA_V, D_MODEL), MLA_HEADS * MLA_V),
        'w_o_b': dense(ks[9], (L, DSA_HEADS * DSA_HEAD_DIM, D_MODEL), DSA_HEADS * DSA_HEAD_DIM),
        'w_out': dense(ks[10], (L, D_MODEL, D_MODEL), D_MODEL),
        'norm_ffn_g': gain(ks[11], (L, D_MODEL)),
        'w_router_group': dense(ks[12], (L, D_MODEL, N_GROUPS), D_MODEL),
        'b_router_group': 0.01 * jax.random.normal(ks[13], (L, N_GROUPS), f32),
        'w_router_expert': dense(ks[14], (L, D_MODEL, N_EXPERTS), D_MODEL),
        'b_router_expert': 0.01 * jax.random.normal(ks[15], (L, N_EXPERTS), f32),
        'w_gate': dense(ks[16], (L, N_EXPERTS, D_MODEL, EXPERT_FF), D_MODEL),
        'w_up': dense(ks[17], (L, N_EXPERTS, D_MODEL, EXPERT_FF), D_MODEL),
        'w_down': dense(ks[18], (L, N_EXPERTS, EXPERT_FF, D_MODEL), EXPERT_FF),
        'final_norm_g': gain(ks[19], (D_MODEL,)),
    }


def reference(x, norm_mix_g, w_in, mla_q_norm_g, w_uq, mla_kv_norm_g, w_uk, w_uv, w_o_a, w_o_b,
              w_out, norm_ffn_g, w_router_group, b_router_group, w_router_expert, b_router_expert,
              w_gate, w_up, w_down, final_norm_g):
    pos = jnp.arange(x.shape[1], dtype=jnp.int32)
    for l in range(DEPTH):
        x = hybrid_layer(x, pos, norm_mix_g[l], w_in[l], mla_q_norm_g[l], w_uq[l], mla_kv_norm_g[l],
                         w_uk[l], w_uv[l], w_o_a[l], w_o_b[l], w_out[l], norm_ffn_g[l],
                         w_router_group[l], b_router_group[l], w_router_expert[l],
                         b_router_expert[l], w_gate[l], w_up[l], w_down[l])
    return rmsnorm(x, final_norm_g)
```

```python
import numpy as np
from contextlib import ExitStack
import concourse.bass as bass
import concourse.mybir as mybir
from concourse.bass_utils import run_bass_kernel_spmd

F32 = mybir.dt.float32
BF16 = mybir.dt.bfloat16
AF = mybir.ActivationFunctionType
ALU = mybir.AluOpType
AX = mybir.AxisListType

S = 8192
D = 1024
NQ = 2048
EPS = 1e-6
THETA = 500000.0
import os as _os0
NITER = int(_os0.environ.get('KNITER', '14'))
TOPK = 256
NEGM = -1.0e30


class Reg:
    __slots__ = ("name", "w", "rs", "rd", "xr", "excl")

    def __init__(self, name, excl=False):
        self.name = name
        self.w = None
        self.rs = {}
        self.rd = []
        self.xr = None
        self.excl = excl


class Op:
    __slots__ = ("eng", "fn", "deps", "need_sig", "sig", "is_dma", "sem_i", "sem_v")


class Prog:
    ENGS = ("pe", "act", "dve", "pool", "sp")
    EPOCH = 8000

    def __init__(self, nc, n_dma_sems=48):
        self.nc = nc
        self.ops = {e: [] for e in self.ENGS}
        self.n_dma_sems = n_dma_sems
        self.dma_last = [None] * n_dma_sems
        self.dma_cnt = [0] * n_dma_sems
        self.dma_rr = 0
        self.dma_ranges = {"sp": (0, 28), "pool": (28, 40), "act": (40, 48)}
        self.dma_rrs = {"sp": 0, "pool": 0, "act": 0}

    def _mk(self, eng, fn, r, w, is_dma, stream):
        self.count = getattr(self, "count", 0) + 1
        if self.count > getattr(self, "limit", 10 ** 9):
            return None
        o = Op()
        o.eng = eng; o.fn = fn; o.need_sig = False; o.sig = None; o.is_dma = is_dma
        o.sem_i = None; o.sem_v = None
        deps = {}
        pb = getattr(self, "pending_barrier", None)
        if pb and pb.get(eng):
            for d in pb.pop(eng):
                if d.is_dma or d.eng != eng:
                    deps[id(d)] = d

        def add(d, raw=False):
            if d is None:
                return
            if d.is_dma or is_dma or d.eng != eng or (raw and eng != "pe" and not stream):
                deps[id(d)] = d
        for reg in r:
            add(reg.w, True)
            if reg.excl:
                add(reg.xr)
        for reg in w:
            add(reg.w)
            for d in reg.rs.values():
                add(d)
            for d in reg.rd:
                add(d)
        if is_dma:
            lo, hi = self.dma_ranges[eng]
            i = lo + self.dma_rrs[eng] % (hi - lo)
            self.dma_rrs[eng] += 1
            prev = self.dma_last[i]
            if prev is not None:
                deps[id(prev)] = prev
            self.dma_cnt[i] += 1
            o.sem_i = i; o.sem_v = 16 * self.dma_cnt[i]
            self.dma_last[i] = o
        o.deps = list(deps.values())
        for d in o.deps:
            if not d.is_dma:
                d.need_sig = True
        for reg in r:
            if is_dma:
                reg.rd.append(o)
            else:
                reg.rs[eng] = o
                if reg.excl:
                    reg.xr = o
        for reg in w:
            reg.w = o; reg.rs = {}; reg.rd = []; reg.xr = None
        self.ops[eng].append(o)
        return o

    def barrier(self):
        lasts = [self.ops[e][-1] for e in self.ENGS if self.ops[e]]
        lasts += [d for d in self.dma_last if d is not None]
        self.pending_barrier = {e: list(lasts) for e in self.ENGS}

    def op(self, eng, fn, r=(), w=(), stream=False):
        return self._mk(eng, fn, r, w, False, stream)

    def dma(self, eng, out, in_, r=(), w=()):
        return self._mk(eng, lambda e: e.dma_start(out=out, in_=in_), r, w, True, False)

    def emit(self):
        nc = self.nc
        final_deps = [d for d in self.dma_last if d is not None]
        nsig = {}
        for eng in self.ENGS:
            c = 0
            for o in self.ops[eng]:
                if o.need_sig and not o.is_dma:
                    c += 1
                    o.sig = c
            nsig[eng] = c
        with ExitStack() as st:
            eng_sems = {}
            for eng in self.ENGS:
                n = nsig[eng] // self.EPOCH + 1
                eng_sems[eng] = [st.enter_context(nc.semaphore(f"s_{eng}_{k}")) for k in range(n)]
            dma_sems = [st.enter_context(nc.semaphore(f"s_dma_{k}")) for k in range(self.n_dma_sems)]
            block = st.enter_context(nc.Block())

            def tok(d):
                if d.is_dma:
                    return dma_sems[d.sem_i], d.sem_v, ("d", d.sem_i)
                k = (d.sig - 1) // self.EPOCH
                return eng_sems[d.eng][k], d.sig - k * self.EPOCH, (d.eng, k)

            def run(eng, e):
                waited = {}
                for o in self.ops[eng]:
                    for d in o.deps:
                        sem, v, key = tok(d)
                        if waited.get(key, 0) < v:
                            e.wait_ge(sem, v)
                            waited[key] = v
                    ins = o.fn(e)
                    if o.is_dma:
                        ins.then_inc(dma_sems[o.sem_i], 16)
                    elif o.need_sig:
                        k = (o.sig - 1) // self.EPOCH
                        ins.then_inc(eng_sems[eng][k], 1)
                if eng == "sp":
                    for d in final_deps:
                        sem, v, key = tok(d)
                        if waited.get(key, 0) < v:
                            e.wait_ge(sem, v)
                            waited[key] = v

            @block.tensor
            def _(e):
                run("pe", e)

            @block.scalar
            def _(e):
                run("act", e)

            @block.vector
            def _(e):
                run("dve", e)

            @block.gpsimd
            def _(e):
                run("pool", e)

            @block.sync
            def _(e):
                run("sp", e)


ARENA_BYTES = 204 * 1024


class Mem:
    def __init__(self, arena):
        self.arena = arena
        self.off = 0
        self.marks = []

    def push(self):
        self.marks.append(self.off)

    def pop(self):
        self.off = self.marks.pop()

    def alloc(self, dt, free_shape, p0=0, p1=128, name=None):
        ap = self.view(self.off, dt, free_shape, p0, p1)
        self.off += self._nb(dt, free_shape)
        return ap

    @staticmethod
    def _nb(dt, free_shape):
        esz = 4 if dt == F32 else 2
        n = 1
        for k in free_shape:
            n *= k
        return (n * esz + 3) // 4 * 4

    def view(self, off, dt, free_shape, p0=0, p1=128):
        esz = 4 if dt == F32 else 2
        n = 1
        for k in free_shape:
            n *= k
        nb = (n * esz + 3) // 4 * 4
        assert off + nb <= ARENA_BYTES, f"SBUF arena overflow {off}+{nb}"
        o4 = off // 4
        ap = self.arena[p0:p1, o4:o4 + nb // 4]
        if dt != F32:
            ap = ap.bitcast(dt)
        if n * esz != nb:
            ap = ap[:, 0:n]
        if len(free_shape) == 2:
            ap = ap.rearrange("p (a b) -> p a b", a=free_shape[0])
        elif len(free_shape) == 3:
            ap = ap.rearrange("p (a b c) -> p a b c", a=free_shape[0], b=free_shape[1])
        return ap


def rope_tables():
    pos = np.arange(S, dtype=np.float32)
    half = 8
    inv = (THETA ** (-np.arange(half, dtype=np.float32) * 2.0 / 16)).astype(np.float32)
    ang = pos[None, :] * inv[:, None]
    c16 = np.ones((128, S), np.float32)
    s16 = np.zeros((128, S), np.float32)
    for base in (0, 64):
        c16[base:base + 8] = np.cos(ang); c16[base + 8:base + 16] = np.cos(ang)
        s16[base:base + 8] = -np.sin(ang); s16[base + 8:base + 16] = np.sin(ang)
    half = 16
    inv = (THETA ** (-np.arange(half, dtype=np.float32) * 2.0 / 32)).astype(np.float32)
    ang = pos[None, :] * inv[:, None]
    c32 = np.concatenate([np.cos(ang), np.cos(ang)], 0).astype(np.float32)
    s32 = np.concatenate([-np.sin(ang), np.sin(ang)], 0).astype(np.float32)
    return np.stack([c16, s16]), np.stack([c32, s32])


def build(debug=False, upto="all"):
    nc = bass.Bass("TRN2", target_bir_lowering=False)
    kind_dbg = "ExternalOutput" if debug else "Internal"

    in_names = []
    nc._in_names = in_names

    def din(name, shape, dt=F32):
        in_names.append(name)
        return nc.dram_tensor(name, list(shape), dt, kind="ExternalInput").ap()

    def dscr(name, shape, dt):
        if debug:
            return nc.dram_tensor(name, list(shape), dt, kind="ExternalOutput").ap()
        return nc.dram_tensor(name, list(shape), dt).ap()

    xb = din("xb", [S, D])
    xq = din("xq", [NQ, D])
    w_in = din("w_in", [D, 4840])
    w_uq = din("w_uq", [384, 768])
    w_uk = din("w_uk", [256, 512])
    w_uv = din("w_uv", [256, 512])
    w_o_a = din("w_o_a", [512, D])
    w_o_b = din("w_o_b", [512, D])
    w_out = din("w_out", [D, D])
    w_rg = din("w_rg", [D, 8])
    w_re = din("w_re", [D, 32])
    b_rg = din("b_rg", [8])
    b_re = din("b_re", [32])
    early = upto in ("W", "N", "A1", "A", "Q", "Q1", "I1", "T1", "T", "F", "F1")
    if not early:
        w_gate = din("w_gate", [32, D, 256])
        w_up = din("w_up", [32, D, 256])
        w_down = din("w_down", [32, 256, D])
    g_mix = din("g_mix", [D])
    g_q = din("g_q", [384])
    g_kv = din("g_kv", [256])
    g_ffn = din("g_ffn", [D])
    g_fin = din("g_fin", [D])
    cs16k = din("cs16k", [2, 128, S])
    cs32k = din("cs32k", [2, 32, S])
    cs16q = din("cs16q", [2, 128, NQ])
    cs32q = din("cs32q", [2, 32, NQ])
    pm16_d = din("pm16", [128, 128])
    pm32_d = din("pm32", [128, 128])
    cs32q4 = din("cs32q4", [2, 128, NQ])
    cmask = din("cmask", [4, 128, 2048])
    cmaskT = din("cmaskT", [128, 16, 512])
    out = nc.dram_tensor("out", [NQ, D], F32, kind="ExternalOutput").ap()

    kbT_d = dscr("kbT_d", [4, 128, S], BF16)
    knT_d = dscr("knT_d", [8, 128, S], BF16)
    vb_d = dscr("vb_d", [S, 1024], BF16)
    va_d = dscr("va_d", [S, 1024], BF16)
    ki_d = dscr("ki_d", [128, S], BF16)
    kpe_d = dscr("kpe_d", [32, S], BF16)
    qb_d = dscr("qb_d", [4, 128, 2048], BF16)
    qi_d = dscr("qi_d", [4, 128, 2048], BF16)
    qn_d = dscr("qn_d", [4, 128, 2048], BF16)
    qpe_d = dscr("qpe_d", [4, 32, 4096], BF16)
    wt_d = dscr("wt_d", [4, 128, 32], F32)
    ga_d = dscr("ga_d", [4, 128, 4096], BF16)
    gb_d = dscr("gb_d", [4, 128, 4096], BF16)
    x1_d = dscr("x1_d", [NQ, D], F32)
    ob_d = dscr("ob_d", [4, 128, 2048], BF16) if debug else None
    oa_d = dscr("oa_d", [4, 128, 2048], BF16) if debug else None
    thr_d = dscr("thr_d", [16, 128, 2], F32) if debug else None
    comb_d = dscr("comb_d", [128, 512], F32) if debug else None
    r_kbT_d, r_knT_d, r_vb_d, r_va_d = Reg("kbT_d"), Reg("knT_d"), Reg("vb_d"), Reg("va_d")
    r_q_d = [Reg(f"q_d{s}") for s in range(4)]
    r_x1_d = Reg("x1_d")
    r_dbg = Reg("dbg")

    P = Prog(nc)
    import os as _os
    P.limit = int(_os.environ.get("KLIMIT", "1000000000"))
    with nc.sbuf_tensor("arena", [128, ARENA_BYTES // 4], F32) as arena, \
            nc.psum_tensor("psum", [128, 4096], F32) as psum:
        M = Mem(arena)
        banks = [psum[:, i * 512:(i + 1) * 512] for i in range(8)]
        r_bank = [Reg(f"bank{i}", excl=True) for i in range(8)]
        rr = {"i": 0, "pool": list(range(8))}

        def nbank():
            pool = rr["pool"]
            i = pool[rr["i"] % len(pool)]
            rr["i"] += 1
            return i

        ident_f = M.alloc(F32, [128]); r_identf = Reg("identf")
        ident = M.alloc(BF16, [128]); r_ident = Reg("ident")
        ones_b = M.alloc(BF16, [128]); r_ones = Reg("ones")
        gmix_b = M.alloc(F32, [D]); r_gmix = Reg("gmix")
        P.op("pool", lambda e: e.memset(ident_f, 1.0), w=[r_identf])
        P.op("pool", lambda e: e.affine_select(out=ident_f, in_=ident_f, pattern=[[-1, 128]],
                                               compare_op=ALU.is_equal, fill=0.0, base=0,
                                               channel_multiplier=1), r=[r_identf], w=[r_identf])
        P.op("pool", lambda e: e.tensor_copy(out=ident, in_=ident_f), r=[r_identf], w=[r_ident])
        P.op("pool", lambda e: e.memset(ones_b, 1.0), w=[r_ones])
        P.dma("sp", gmix_b, g_mix.partition_broadcast(128), w=[r_gmix])
        pm16 = M.alloc(BF16, [128]); r_pm16 = Reg("pm16")
        pm32 = M.alloc(BF16, [128]); r_pm32 = Reg("pm32")
        P.dma("pool", pm16, pm16_d, w=[r_pm16])
        P.dma("pool", pm32, pm32_d, w=[r_pm32])

        r_ki_d, r_kpe_d = Reg("ki_d"), Reg("kpe_d")

        def norm_transpose(src_rows, gb_ap, r_gb, bufs, hT, r_hT, blk_tag, part="both"):
            G = len(bufs)
            for g0 in range(0, 4, G):
                grp = list(range(g0, min(4, g0 + G)))
                for t in (grp if part in ("both", "stats") else []):
                    xt, r_xt, xn, r_xn, st, r_st, jk, r_jk = bufs[t % G]
                    P.dma("sp", xt, src_rows(t), w=[r_xt])
                    P.op("act", lambda e, xt=xt, jk=jk, st=st: e.activation(
                        out=jk, in_=xt, func=AF.Square, accum_out=st[:, 0:1]), r=[r_xt], w=[r_jk, r_st])
                    P.op("act", lambda e, st=st: e.activation(
                        out=st[:, 1:2], in_=st[:, 0:1], func=AF.Sqrt, scale=1.0 / D, bias=EPS),
                        r=[r_st], w=[r_st])
                for t in (grp if part in ("both", "stats") else []):
                    xt, r_xt, xn, r_xn, st, r_st, jk, r_jk = bufs[t % G]
                    P.op("dve", lambda e, st=st: e.reciprocal(out=st[:, 2:3], in_=st[:, 1:2]),
                         r=[r_st], w=[r_st])
                    P.op("dve", lambda e, xn=xn, xt=xt, st=st: e.scalar_tensor_tensor(
                        out=xn, in0=xt, scalar=st[:, 2:3], in1=gb_ap, op0=ALU.mult, op1=ALU.mult),
                        r=[r_xt, r_st, r_gb], w=[r_xn])
                pend = []
                for t in (grp if part in ("both", "T") else []):
                    xt, r_xt, xn, r_xn, st, r_st, jk, r_jk = bufs[t % G]
                    b = nbank()
                    tp = banks[b].bitcast(BF16)
                    for c in range(8):
                        P.op("pe", lambda e, tp=tp, xn=xn, c=c: e.transpose(
                            out=tp[:, c * 128:(c + 1) * 128], in_=xn[:, c * 128:(c + 1) * 128],
                            identity=ident), r=[r_xn, r_ident], w=[r_bank[b]])
                    pend.append((t, b, tp))
                for (t, b, tp) in pend:
                    src = tp.rearrange("p (c k) -> p c k", c=8)
                    dst = hT[:, :, t * 128:(t + 1) * 128]
                    evac("act" if t % 2 == 0 else "dve", dst, src, [r_bank[b]], [r_hT])

        def mk_xbufs(n=2):
            bufs = []
            for i in range(n):
                xt = M.alloc(F32, [D]); xn = M.alloc(BF16, [D]); st = M.alloc(F32, [4])
                jk = M.alloc(BF16, [D])
                bufs.append((xt, Reg(f"xt{i}"), xn, Reg(f"xn{i}"), st, Reg(f"st{i}"), jk, Reg(f"jk{i}")))
            return bufs

        def evac(eng, dst, src, r, w, scale=None, func=None):
            if eng == "act":
                if scale is None:
                    P.op("act", lambda e: e.activation(out=dst, in_=src, func=func or AF.Copy), r=r, w=w)
                else:
                    P.op("act", lambda e: e.activation(out=dst, in_=src, func=func or AF.Copy, scale=scale),
                         r=r, w=w)
            else:
                if scale is None:
                    P.op("dve", lambda e: e.tensor_copy(out=dst, in_=src), r=r, w=w)
                else:
                    P.op("dve", lambda e: e.tensor_scalar(out=dst, in0=src, scalar1=scale, scalar2=None,
                                                          op0=ALU.mult), r=r, w=w)

        def proj_T(wt, r_wt, col0, m, hT, r_hT, nchunk, p1=None):
            b = nbank()
            o = banks[b][0:m, :]
            for c in range(nchunk):
                P.op("pe", lambda e, o=o, c=c: e.matmul(
                    o, lhsT=wt[:, c, col0:col0 + m], rhs=hT[:, c, :], start=(c == 0), stop=(c == nchunk - 1)),
                    r=[r_wt, r_hT], w=[r_bank[b]])
            return b

        def rope_combine(b0, b1, m, cos, sin, r_cs, dst, r_dst, t1, r_t1, t2, r_t2, scale=None):
            a = banks[b0][0:m, :]; bb = banks[b1][0:m, :]
            if scale is None:
                P.op("dve", lambda e: e.tensor_tensor(out=t1, in0=a, in1=cos, op=ALU.mult),
                     r=[r_bank[b0], r_cs], w=[r_t1])
                P.op("dve", lambda e: e.tensor_tensor(out=t2, in0=bb, in1=sin, op=ALU.mult),
                     r=[r_bank[b1], r_cs], w=[r_t2])
            else:
                P.op("dve", lambda e: e.scalar_tensor_tensor(out=t1, in0=a, scalar=scale, in1=cos,
                                                             op0=ALU.mult, op1=ALU.mult),
                     r=[r_bank[b0], r_cs], w=[r_t1])
                P.op("dve", lambda e: e.scalar_tensor_tensor(out=t2, in0=bb, scalar=scale, in1=sin,
                                                             op0=ALU.mult, op1=ALU.mult),
                     r=[r_bank[b1], r_cs], w=[r_t2])
            P.op("pool", lambda e: e.tensor_tensor(out=dst, in0=t1, in1=t2, op=ALU.add),
                 r=[r_t1, r_t2], w=[r_dst])

        def rope_perm(b0, m, pm, r_pm, cos, sin, r_cs, dst, r_dst, xo, r_xo, t1, r_t1, t2, r_t2, scale=None):
            evac("act", xo[0:m, :], banks[b0][0:m, :], [r_bank[b0]], [r_xo])

            def finish():
                b1 = nbank()
                P.op("pe", lambda e: e.matmul(banks[b1][0:m, :], lhsT=pm[0:m, 0:m], rhs=xo[0:m, :], start=True, stop=True),
                     r=[r_xo, r_pm], w=[r_bank[b1]])
                rope_combine(b0, b1, m, cos, sin, r_cs, dst, r_dst, t1, r_t1, t2, r_t2, scale=scale)
            return finish

        def make_swapped(eng, wt, r_wt, src0, dst0, nheads, hd, half, nchunk):
            n = nheads * hd
            P.op(eng, lambda e: e.tensor_copy(out=wt[:, :, dst0:dst0 + n], in_=wt[:, :, src0:src0 + n]),
                 r=[r_wt], w=[r_wt])
            sv = wt[:, :, src0:src0 + n].rearrange("p c (h d) -> p c h d", h=nheads)
            dv = wt[:, :, dst0:dst0 + n].rearrange("p c (h d) -> p c h d", h=nheads)
            for c in range(nchunk):
                P.op(eng, lambda e, c=c: e.tensor_copy(out=dv[:, c, :, 0:half], in_=sv[:, c, :, half:2 * half]),
                     r=[r_wt], w=[r_wt])
                P.op(eng, lambda e, c=c: e.tensor_copy(out=dv[:, c, :, half:2 * half], in_=sv[:, c, :, 0:half]),
                     r=[r_wt], w=[r_wt])

        w_in_v = w_in.rearrange("(c p) n -> p c n", p=128)

        def load_cols(dst, r_dst, vec, nchunk):
            v = vec.rearrange("(c p o) -> c p o", p=128, o=1)
            for c in range(nchunk):
                P.dma("sp", dst[:, c:c + 1], v[c], w=[r_dst])

        M.push()
        kiT2 = M.alloc(BF16, [S]); r_kiT2 = Reg("kiT2")
        kpeT = M.alloc(BF16, [S], 0, 32); r_kpeT = Reg("kpeT")
        if debug:
            P.op("pool", lambda e: e.memset(kiT2, 0.0), w=[r_kiT2])
            P.op("pool", lambda e: e.memset(kpeT, 0.0), w=[r_kpeT])
        NK = 2112
        Wk = M.alloc(BF16, [8, NK]); r_Wk = Reg("Wk")
        Wuk = M.alloc(BF16, [2, 512]); r_Wuk = Reg("Wuk")
        Wuv = M.alloc(BF16, [2, 512]); r_Wuv = Reg("Wuv")
        gkv = M.alloc(F32, [2]); r_gkv = Reg("gkv")
        C_CKV, C_KR, C_KRS, C_KB, C_KBS, C_VB, C_KI, C_KIS = 0, 256, 288, 320, 832, 1344, 1856, 1984
        for (dst, src, n) in ((C_CKV, 384, 256), (C_KR, 640, 32), (C_KB, 1184, 512), (C_VB, 1696, 512),
                              (C_KI, 2720, 64), (C_KI + 64, 2720, 64)):
            for c0 in range(0, 8, 4):
                P.dma("pool", Wk[:, c0:c0 + 4, dst:dst + n], w_in_v[:, c0:c0 + 4, src:src + n], w=[r_Wk])
        P.dma("pool", Wuk, w_uk.rearrange("(c p) n -> p c n", p=128), w=[r_Wuk])
        P.dma("pool", Wuv, w_uv.rearrange("(c p) n -> p c n", p=128), w=[r_Wuv])
        load_cols(gkv, r_gkv, g_kv, 2)
        xos = [(M.alloc(BF16, [512]), Reg(f"xoA{i}")) for i in range(2)]

        if upto == "W":
            P.dma("sp", kbT_d[0, :, 0:NK * 2], Wk[:, 0:2, :].rearrange("p c n -> p (c n)"), r=[r_Wk], w=[r_dbg])
            P.emit()
            return nc
        zt = M.alloc(BF16, [S], 0, 32); r_zt = Reg("zt")
        P.op("pool", lambda e: e.memset(zt, 0.0), w=[r_zt])
        for h8 in range(8):
            P.dma("sp", knT_d[h8, 96:128, :], zt, r=[r_zt], w=[r_knT_d])
        xbufs = mk_xbufs(4)
        hTs = [(M.alloc(BF16, [8, 512]), Reg(f"hT{i}")) for i in range(2)]
        cs16 = [(M.alloc(F32, [2, 512]), Reg(f"cs16_{i}")) for i in range(2)]
        cs32 = [(M.alloc(F32, [2, 512], 0, 32), Reg(f"cs32_{i}")) for i in range(2)]
        ckvf = M.alloc(F32, [2, 512]); r_ckvf = Reg("ckvf")
        sq = M.alloc(BF16, [2, 512]); r_sq = Reg("sq")
        sd = M.alloc(F32, [512]); r_sd = Reg("sd")
        rstdb = M.alloc(F32, [512]); r_rstdb = Reg("rstdb")
        ckvn = M.alloc(BF16, [2, 512]); r_ckvn = Reg("ckvn")
        t1s = [(M.alloc(F32, [512]), Reg(f"t1_{i}")) for i in range(2)]
        t2s = [(M.alloc(F32, [512]), Reg(f"t2_{i}")) for i in range(2)]
        obufs = [(M.alloc(BF16, [512]), Reg(f"ob{i}")) for i in range(4)]
        vaugs = [(M.alloc(BF16, [4, 8, 128]), Reg(f"vaug{i}")) for i in range(2)]
        for va_, r_va in vaugs:
            P.op("pool", lambda e, va_=va_: e.memset(va_, 1.0), w=[r_va])
        cnt = {"ob": 0, "t": 0}

        def next_ob():
            cnt["ob"] += 1
            return obufs[cnt["ob"] % 4]

        def next_t():
            cnt["t"] += 1
            return t1s[cnt["t"] % 2] + t2s[cnt["t"] % 2]

        def flush_pend(pend, tok0, keep):
            while len(pend) > keep:
                kind, fin, kb_info, _ = pend.pop(0)
                fin()
                if kind == "kr":
                    for h8 in range(8):
                        P.dma("sp", knT_d[h8, 64:96, tok0:tok0 + 512], kpeT[:, tok0:tok0 + 512], r=[r_kpeT], w=[r_knT_d])
                else:
                    hp_, ob_, r_ob_ = kb_info
                    P.dma("sp", kbT_d[hp_, :, tok0:tok0 + 512], ob_, r=[r_ob_], w=[r_kbT_d])

        xb_v = xb.rearrange("(n p) d -> n p d", p=128)
        nblk_a = 16 if upto != "A1" else 1
        if _os.environ.get("KSKIPA"):
            nblk_a = 0
        if _os.environ.get("KNBLKA"):
            nblk_a = int(_os.environ["KNBLKA"])
        MOEONLY = bool(_os.environ.get("KMOEONLY"))
        if MOEONLY:
            nblk_a = 0
        def blockA_pre(blk, part="both"):
            hT, r_hT = hTs[blk % 2]
            c16, r_c16 = cs16[blk % 2]
            c32, r_c32 = cs32[blk % 2]
            tok0 = blk * 512
            if part in ("both", "stats"):
                P.dma("sp", c16, cs16k[:, :, tok0:tok0 + 512].rearrange("a p n -> p a n"), w=[r_c16])
                P.dma("sp", c32, cs32k[:, :, tok0:tok0 + 512].rearrange("a p n -> p a n"), w=[r_c32])
            norm_transpose(lambda t: xb_v[blk * 4 + t], gmix_b, r_gmix, xbufs, hT, r_hT, blk, part=part)

        def blockA_proj(blk):
            hT, r_hT = hTs[blk % 2]
            c16, r_c16 = cs16[blk % 2]
            c32, r_c32 = cs32[blk % 2]
            tok0 = blk * 512
            for cc in range(2):
                b = proj_T(Wk, r_Wk, C_CKV + cc * 128, 128, hT, r_hT, 8)
                P.op("act", lambda e, b=b, cc=cc: e.activation(out=sq[:, cc, :], in_=banks[b], func=AF.Square),
                     r=[r_bank[b]], w=[r_sq])
                P.op("dve", lambda e, b=b, cc=cc: e.tensor_copy(out=ckvf[:, cc, :], in_=banks[b]),
                     r=[r_bank[b]], w=[r_ckvf])
            b = nbank()
            for cc in range(2):
                P.op("pe", lambda e, b=b, cc=cc: e.matmul(banks[b], lhsT=ones_b, rhs=sq[:, cc, :],
                                                          start=(cc == 0), stop=(cc == 1)),
                     r=[r_ones, r_sq], w=[r_bank[b]])
            P.op("act", lambda e, b=b: e.activation(out=sd, in_=banks[b], func=AF.Sqrt, scale=1.0 / 256, bias=EPS),
                 r=[r_bank[b]], w=[r_sd])
            P.op("dve", lambda e: e.reciprocal(out=rstdb, in_=sd), r=[r_sd], w=[r_rstdb])
            for cc in range(2):
                P.op("dve", lambda e, cc=cc: e.scalar_tensor_tensor(
                    out=ckvn[:, cc, :], in0=ckvf[:, cc, :], scalar=gkv[:, cc:cc + 1], in1=rstdb,
                    op0=ALU.mult, op1=ALU.mult), r=[r_ckvf, r_gkv, r_rstdb], w=[r_ckvn])
            b0 = proj_T(Wk, r_Wk, C_KR, 32, hT, r_hT, 8)
            t1, r_t1, t2, r_t2 = next_t()
            xo, r_xo = xos[0]
            fin_kr = rope_perm(b0, 32, pm32, r_pm32, c32[:, 0, :], c32[:, 1, :], r_c32, kpeT[:, tok0:tok0 + 512], r_kpeT,
                               xo, r_xo, t1[0:32, :], r_t1, t2[0:32, :], r_t2)
            pend = [("kr", fin_kr, None, None)]
            for hp in range(4):
                b0 = proj_T(Wk, r_Wk, C_KB + hp * 128, 128, hT, r_hT, 8)
                ob, r_ob = next_ob()
                t1, r_t1, t2, r_t2 = next_t()
                xo, r_xo = xos[(hp + 1) % 2]
                fin = rope_perm(b0, 128, pm16, r_pm16, c16[:, 0, :], c16[:, 1, :], r_c16, ob, r_ob, xo, r_xo, t1, r_t1, t2, r_t2)
                pend.append(("kb", fin, (hp, ob, r_ob), None))
                flush_pend(pend, tok0, 1)
            if blk + 1 < nblk_a:
                blockA_pre(blk + 1, part="stats")
            vb_, r_vb = vaugs[1]
            for t in range(4):
                b = nbank()
                for c in range(8):
                    P.op("pe", lambda e, b=b, c=c, t=t, hT=hT: e.matmul(
                        banks[b], lhsT=hT[:, c, t * 128:(t + 1) * 128], rhs=Wk[:, c, C_VB:C_VB + 512],
                        start=(c == 0), stop=(c == 7)), r=[r_hT, r_Wk], w=[r_bank[b]])
                src4 = banks[b].rearrange("p (hp hh e) -> p hp hh e", hh=2, e=64)
                dst4 = vb_[:, t, :, :].rearrange("p (hp hh) e -> p hp hh e", hh=2)
                evac("act", dst4[:, :, 0, 0:64], src4[:, :, 0, :], [r_bank[b]], [r_vb])
                evac("dve", dst4[:, :, 1, 64:128], src4[:, :, 1, :], [r_bank[b]], [r_vb])
            P.dma("sp", vb_d[tok0:tok0 + 512, :].rearrange("(t p) n -> p t n", p=128),
                  vb_.rearrange("p t h e -> p t (h e)"), r=[r_vb], w=[r_vb_d])
            b0 = proj_T(Wk, r_Wk, C_KI, 128, hT, r_hT, 8)
            t1, r_t1, t2, r_t2 = next_t()
            flush_pend(pend, tok0, 0)
            xo, r_xo = xos[0]
            fin = rope_perm(b0, 128, pm16, r_pm16, c16[:, 0, :], c16[:, 1, :], r_c16, kiT2[:, tok0:tok0 + 512], r_kiT2,
                            xo, r_xo, t1, r_t1, t2, r_t2)
            fin()
            for hp in range(4):
                b = proj_T(Wuk, r_Wuk, hp * 128, 128, ckvn, r_ckvn, 2)
                ob, r_ob = next_ob()
                evac("act", ob, banks[b], [r_bank[b]], [r_ob])
                P.dma("sp", knT_d[2 * hp, 0:64, tok0:tok0 + 512], ob[0:64, :], r=[r_ob], w=[r_knT_d])
                P.dma("sp", knT_d[2 * hp + 1, 0:64, tok0:tok0 + 512], ob[64:128, :], r=[r_ob], w=[r_knT_d])
            va_, r_va = vaugs[0]
            for t in range(4):
                b = nbank()
                for cc in range(2):
                    P.op("pe", lambda e, b=b, cc=cc, t=t: e.matmul(
                        banks[b], lhsT=ckvn[:, cc, t * 128:(t + 1) * 128], rhs=Wuv[:, cc, :],
                        start=(cc == 0), stop=(cc == 1)), r=[r_ckvn, r_Wuv], w=[r_bank[b]])
                src4 = banks[b].rearrange("p (hp hh e) -> p hp hh e", hh=2, e=64)
                dst4 = va_[:, t, :, :].rearrange("p (hp hh) e -> p hp hh e", hh=2)
                evac("act", dst4[:, :, 0, 0:64], src4[:, :, 0, :], [r_bank[b]], [r_va])
                evac("dve", dst4[:, :, 1, 64:128], src4[:, :, 1, :], [r_bank[b]], [r_va])
            P.dma("sp", va_d[tok0:tok0 + 512, :].rearrange("(t p) n -> p t n", p=128),
                  va_.rearrange("p t h e -> p t (h e)"), r=[r_va], w=[r_va_d])

        if nblk_a > 0:
            blockA_pre(0)
        for blk in range(nblk_a):
            blockA_proj(blk)
            if blk + 1 < nblk_a:
                blockA_pre(blk + 1, part="T")
        P.dma("sp", ki_d, kiT2, r=[r_kiT2], w=[r_ki_d])
        P.dma("sp", kpe_d, kpeT, r=[r_kpeT], w=[r_kpe_d])
        M.pop()
        P.barrier()
        if upto in ("A", "A1"):
            P.emit()
            return nc


        M.push()
        NQW = 4488
        Wq = M.alloc(BF16, [8, NQW]); r_Wq = Reg("Wq")
        Wun = M.alloc(BF16, [3, 512]); r_Wun = Reg("Wun")
        Wur = M.alloc(BF16, [3, 512]); r_Wur = Reg("Wur")
        gq = M.alloc(F32, [3]); r_gq = Reg("gq")
        Q_CQ, Q_QB, Q_QBS, Q_QI, Q_QIS, Q_WI, Q_GA, Q_GB = 0, 384, 896, 1408, 1920, 2432, 2440, 3464
        for (dst, src, n) in ((Q_CQ, 0, 384), (Q_QB, 672, 512), (Q_QI, 2208, 512), (Q_WI, 2784, 8),
                              (Q_GA, 2792, 1024), (Q_GB, 3816, 1024)):
            for c0 in range(0, 8, 4):
                P.dma("pool", Wq[:, c0:c0 + 4, dst:dst + n], w_in_v[:, c0:c0 + 4, src:src + n], w=[r_Wq])
        w_uq_v = w_uq.rearrange("(c p) (h e) -> p c h e", p=128, h=8)
        for c in range(3):
            P.dma("pool", Wun[:, c, :].rearrange("p (h e) -> p h e", h=8), w_uq_v[:, c, :, 0:64], w=[r_Wun])
            P.dma("pool", Wur[:, c, 0:256].rearrange("p (h e) -> p h e", h=8), w_uq_v[:, c, :, 64:96], w=[r_Wur])
        load_cols(gq, r_gq, g_q, 3)
        xoq = [(M.alloc(BF16, [512]), Reg(f"xoQ{i}")) for i in range(2)]
        c32q4 = M.alloc(F32, [2, 512]); r_c32q4 = Reg("c32q4")
        xbufs = mk_xbufs(4)
        hTq = M.alloc(BF16, [8, 512]); r_hTq = Reg("hTq")
        c16q = M.alloc(F32, [2, 512]); r_c16q = Reg("c16q")
        c32q = M.alloc(F32, [2, 512], 0, 32); r_c32q = Reg("c32q")
        cqf = M.alloc(F32, [3, 512]); r_cqf = Reg("cqf")
        sq3 = M.alloc(BF16, [3, 512]); r_sq3 = Reg("sq3")
        sdq = M.alloc(F32, [512]); r_sdq = Reg("sdq")
        rstdq = M.alloc(F32, [512]); r_rstdq = Reg("rstdq")
        cqn = M.alloc(BF16, [3, 512]); r_cqn = Reg("cqn")
        t1s = [(M.alloc(F32, [512]), Reg(f"qt1_{i}")) for i in range(2)]
        t2s = [(M.alloc(F32, [512]), Reg(f"qt2_{i}")) for i in range(2)]
        qb_t = M.alloc(BF16, [4, 512]); r_qb_t = Reg("qb_t")
        qi_t = M.alloc(BF16, [4, 512]); r_qi_t = Reg("qi_t")
        qn_t = M.alloc(BF16, [4, 512]); r_qn_t = Reg("qn_t")
        qpe_t = M.alloc(BF16, [2, 512]); r_qpe_t = Reg("qpe_t")
        wt_t = M.alloc(F32, [4, 8]); r_wt_t = Reg("wt_t")
        g_t = [(M.alloc(BF16, [8, 512]), Reg(f"g_t{i}")) for i in range(2)]
        xq_v = xq.rearrange("(n p) d -> n p d", p=128)
        SC_A = 96 ** -0.5
        for s in range(0 if MOEONLY else (4 if (upto != "Q1" and not _os.environ.get("KQ1")) else 1)):
            q0 = s * 512
            P.dma("sp", c16q, cs16q[:, :, q0:q0 + 512].rearrange("a p n -> p a n"), w=[r_c16q])
            P.dma("sp", c32q4, cs32q4[:, :, q0:q0 + 512].rearrange("a p n -> p a n"), w=[r_c32q4])
            norm_transpose(lambda t, s=s: xq_v[s * 4 + t], gmix_b, r_gmix, xbufs, hTq, r_hTq, s)
            for cc in range(3):
                b = proj_T(Wq, r_Wq, Q_CQ + cc * 128, 128, hTq, r_hTq, 8)
                P.op("act", lambda e, b=b, cc=cc: e.activation(out=sq3[:, cc, :], in_=banks[b], func=AF.Square),
                     r=[r_bank[b]], w=[r_sq3])
                P.op("dve", lambda e, b=b, cc=cc: e.tensor_copy(out=cqf[:, cc, :], in_=banks[b]),
                     r=[r_bank[b]], w=[r_cqf])
            b = nbank()
            for cc in range(3):
                P.op("pe", lambda e, b=b, cc=cc: e.matmul(banks[b], lhsT=ones_b, rhs=sq3[:, cc, :],
                                                          start=(cc == 0), stop=(cc == 2)),
                     r=[r_ones, r_sq3], w=[r_bank[b]])
            P.op("act", lambda e, b=b: e.activation(out=sdq, in_=banks[b], func=AF.Sqrt, scale=1.0 / 384, bias=EPS),
                 r=[r_bank[b]], w=[r_sdq])
            P.op("dve", lambda e: e.reciprocal(out=rstdq, in_=sdq), r=[r_sdq], w=[r_rstdq])
            for cc in range(3):
                P.op("dve", lambda e, cc=cc: e.scalar_tensor_tensor(
                    out=cqn[:, cc, :], in0=cqf[:, cc, :], scalar=gq[:, cc:cc + 1], in1=rstdq,
                    op0=ALU.mult, op1=ALU.mult), r=[r_cqf, r_gq, r_rstdq], w=[r_cqn])
            qpend = []
            items = [("qb", hp) for hp in range(4)] + [("qi", hp) for hp in range(4)] + [("pe", g4) for g4 in range(2)]
            for ii, (kind, j) in enumerate(items):
                t1, r_t1 = t1s[ii % 2]; t2, r_t2 = t2s[ii % 2]
                xo, r_xo = xoq[ii % 2]
                if kind == "pe":
                    b0 = proj_T(Wur, r_Wur, j * 128, 128, cqn, r_cqn, 3)
                    fin = rope_perm(b0, 128, pm32, r_pm32, c32q4[:, 0, :], c32q4[:, 1, :], r_c32q4, qpe_t[:, j, :], r_qpe_t,
                                    xo, r_xo, t1, r_t1, t2, r_t2, scale=SC_A)
                elif kind == "qb":
                    b0 = proj_T(Wq, r_Wq, Q_QB + j * 128, 128, hTq, r_hTq, 8)
                    fin = rope_perm(b0, 128, pm16, r_pm16, c16q[:, 0, :], c16q[:, 1, :], r_c16q, qb_t[:, j, :], r_qb_t,
                                    xo, r_xo, t1, r_t1, t2, r_t2, scale=0.125)
                else:
                    b0 = proj_T(Wq, r_Wq, Q_QI + j * 128, 128, hTq, r_hTq, 8)
                    fin = rope_perm(b0, 128, pm16, r_pm16, c16q[:, 0, :], c16q[:, 1, :], r_c16q, qi_t[:, j, :], r_qi_t,
                                    xo, r_xo, t1, r_t1, t2, r_t2)
                qpend.append(fin)
                if len(qpend) > 1:
                    qpend.pop(0)()
            while qpend:
                qpend.pop(0)()
            for t in range(4):
                b = nbank()
                for c in range(8):
                    P.op("pe", lambda e, b=b, c=c, t=t: e.matmul(
                        banks[b][:, 0:8], lhsT=hTq[:, c, t * 128:(t + 1) * 128], rhs=Wq[:, c, Q_WI:Q_WI + 8],
                        start=(c == 0), stop=(c == 7)), r=[r_hTq, r_Wq], w=[r_bank[b]])
                evac("act", wt_t[:, t, :], banks[b][:, 0:8], [r_bank[b]], [r_wt_t], scale=(8 ** -0.5) * 0.125)
            for which in range(2):
                gt, r_gt = g_t[which]
                for dch in range(8):
                    b = proj_T(Wq, r_Wq, (Q_GA, Q_GB)[which] + dch * 128, 128, hTq, r_hTq, 8)
                    evac("act", gt[:, dch, :], banks[b], [r_bank[b]], [r_gt], func=AF.Sigmoid)
            for hp in range(4):
                b = proj_T(Wun, r_Wun, hp * 128, 128, cqn, r_cqn, 3)
                evac("act", qn_t[:, hp, :], banks[b], [r_bank[b]], [r_qn_t], scale=SC_A)
            P.dma("pool", qb_d[s], qb_t.rearrange("p a n -> p (a n)"), r=[r_qb_t], w=[r_q_d[s]])
            P.dma("pool", qi_d[s], qi_t.rearrange("p a n -> p (a n)"), r=[r_qi_t], w=[r_q_d[s]])
            P.dma("pool", qn_d[s], qn_t.rearrange("p a n -> p (a n)"), r=[r_qn_t], w=[r_q_d[s]])
            for h8 in range(8):
                g4, hl = divmod(h8, 4)
                P.dma("pool", qpe_d[s, :, h8 * 512:(h8 + 1) * 512], qpe_t[hl * 32:(hl + 1) * 32, g4, :],
                      r=[r_qpe_t], w=[r_q_d[s]])
            P.dma("pool", wt_d[s], wt_t.rearrange("p a n -> p (a n)"), r=[r_wt_t], w=[r_q_d[s]])
            P.dma("pool", ga_d[s], g_t[0][0].rearrange("p a n -> p (a n)"), r=[g_t[0][1]], w=[r_q_d[s]])
            P.dma("pool", gb_d[s], g_t[1][0].rearrange("p a n -> p (a n)"), r=[g_t[1][1]], w=[r_q_d[s]])
        M.pop()
        P.barrier()
        if upto in ("Q", "Q1"):
            P.emit()
            return nc

        M.push()
        kiT2 = M.alloc(BF16, [S]); r_kiT2 = Reg("kiT2b")
        P.dma("sp", kiT2, ki_d, r=[r_ki_d], w=[r_kiT2])
        qi_s = M.alloc(BF16, [8, 512]); r_qi_s = Reg("qi_s")
        P.op("pool", lambda e: e.memset(qi_s, 0.0), w=[r_qi_s])
        wt_s = M.alloc(F32, [4, 8]); r_wt_s = Reg("wt_s")
        o_a = M.alloc(BF16, [4, 512]); r_o_a = Reg("o_a")
        o_b = M.alloc(BF16, [4, 512]); r_o_b = Reg("o_b")
        pw2 = M.alloc(F32, [NITER]); r_pw2 = Reg("pw2")
        bis = M.alloc(F32, [8 + NITER]); r_bis = Reg("bis")
        bisB = M.alloc(F32, [4]); r_bisB = Reg("bisB")
        bisA = M.alloc(F32, [4]); r_bisA = Reg("bisA")
        selm = M.alloc(F32, [128]); r_selm = Reg("selm")
        P.op("pool", lambda e: e.memset(selm, 0.0), w=[r_selm])
        P.op("pool", lambda e: e.memset(selm[0:1, :], 1.0), r=[r_selm], w=[r_selm])
        P.op("pool", lambda e: e.memset(selm[64:65, :], 1.0), r=[r_selm], w=[r_selm])
        att_ev = [(M.alloc(F32, [512]), Reg(f"att_ev{i}")) for i in range(2)]
        att_rs = [(M.alloc(F32, [512]), Reg(f"att_rs{i}")) for i in range(2)]
        for k in range(NITER):
            P.op("pool", lambda e, k=k: e.memset(pw2[:, k:k + 1], 2.0 ** (-k)), w=[r_pw2])
        rr["pool"] = [0, 1, 2, 3, 4, 5]
        kvstate = {"n": 0}
        nslots = 4 if upto not in ("I1", "T1", "F1") else 1
        if MOEONLY:
            nslots = 0
        if _os.environ.get("KNSLOT"):
            nslots = int(_os.environ["KNSLOT"])

        def attention(s, hp, kT_d, r_kT_d, v_d, r_v_d, q_s, r_q_s, o_t, r_o_t, mla, maskT, r_maskT, kbufs, vbufs, pts, cmT, r_cmT, kpeT=None, r_kpeT=None, qpe_s=None, r_qpe_s=None):
            tiles = [(c, t, hh) for c in range(s + 1) for t in range(16) for hh in range(2)]
            loaded = {}
            state = {"n": 0}

            def load(c):
                if c in loaded:
                    return loaded[c]
                kvi = kvstate["n"] % len(kbufs); kvstate["n"] += 1
                kb_, r_kb_ = kbufs[kvi]; vb_, r_vb_ = vbufs[kvi]
                if mla:
                    P.dma("sp", kb_, kT_d[2 * hp:2 * hp + 2, :, c * 2048:(c + 1) * 2048].rearrange("h p n -> p h n"),
                          r=[r_kT_d], w=[r_kb_])
                else:
                    P.dma("sp", kb_[:, 0, :], kT_d[hp, :, c * 2048:(c + 1) * 2048], r=[r_kT_d], w=[r_kb_])
                P.dma("sp", vb_, v_d[c * 2048:(c + 1) * 2048, hp * 256:(hp + 1) * 256].rearrange(
                    "(t p) e -> p t e", p=128), r=[r_v_d], w=[r_vb_])
                loaded[c] = (kb_, r_kb_, vb_, r_vb_)
                return loaded[c]

            def stage1(i):
                c, t, hh = tiles[i]
                kt = c * 16 + t
                kb_, r_kb_, vb_, r_vb_ = load(c)
                b = nbank()
                pt, r_pt = pts[i % len(pts)]
                hsel = hh if mla else 0
                P.op("pe", lambda e: e.matmul(
                    banks[b], lhsT=kb_[:, hsel, t * 128:(t + 1) * 128],
                    rhs=q_s[:, hp * 2 + hh, :], start=True, stop=True),
                    r=[r_kb_, r_q_s], w=[r_bank[b]])
                P.op("act", lambda e: e.activation(out=pt, in_=banks[b], func=AF.Exp), r=[r_bank[b]], w=[r_pt])
                eng = "dve" if i % 2 == 0 else "pool"
                if not mla:
                    P.op(eng, lambda e: e.tensor_tensor(out=pt, in0=pt, in1=maskT[:, kt, :], op=ALU.mult),
                         r=[r_pt, r_maskT], w=[r_pt])
                elif c == s:
                    P.op(eng, lambda e: e.tensor_tensor(out=pt, in0=pt, in1=cmT[:, t, :], op=ALU.mult),
                         r=[r_pt, r_cmT], w=[r_pt])

            def stage2(i):
                c, t, hh = tiles[i]
                kb_, r_kb_, vb_, r_vb_ = loaded[c]
                pt, r_pt = pts[i % len(pts)]
                first = (c == 0 and t == 0)
                last = (c == s and t == 15)
                P.op("pe", lambda e: e.matmul(
                    banks[6 + hh], lhsT=vb_[:, t, hh * 128:(hh + 1) * 128], rhs=pt, start=first, stop=last),
                    r=[r_pt, r_vb_], w=[r_bank[6 + hh]])

            DEPTH = 6
            n = len(tiles)
            for i in range(0, n + DEPTH, 2):
                for j in (i, i + 1):
                    if j < n:
                        stage1(j)
                for j in (i - DEPTH, i - DEPTH + 1):
                    if 0 <= j < n:
                        stage2(j)
            for hh in range(2):
                srow = slice(64, 128) if hh == 0 else slice(0, 64)
                orow = slice(0, 64) if hh == 0 else slice(64, 128)
                evs, r_evs = att_ev[hh]
                rsb, r_rsb = att_rs[hh]
                P.op("act", lambda e, hh=hh, srow=srow, evs=evs: e.activation(
                    out=evs[srow, :], in_=banks[6 + hh][srow, :], func=AF.Copy), r=[r_bank[6 + hh]], w=[r_evs])
                b = nbank()
                P.op("pe", lambda e, b=b, srow=srow, evs=evs: e.matmul(
                    banks[b], lhsT=selm[srow, :], rhs=evs[srow, :], start=True, stop=True),
                    r=[r_evs, r_selm], w=[r_bank[b]])
                P.op("dve", lambda e, b=b, orow=orow, rsb=rsb: e.reciprocal(out=rsb[orow, :], in_=banks[b][orow, :]),
                     r=[r_bank[b]], w=[r_rsb])
                P.op("dve", lambda e, hh=hh, orow=orow, rsb=rsb: e.tensor_tensor(
                    out=o_t[orow, hp, :], in0=banks[6 + hh][orow, :], in1=rsb[orow, :], op=ALU.mult),
                    r=[r_bank[6 + hh], r_rsb], w=[r_o_t])

        def do_slot(s):
            ext = 2048 * (s + 1)
            nkt = 16 * (s + 1)
            for hh_ in range(2):
                rws = slice(hh_ * 64, (hh_ + 1) * 64)
                P.dma("sp", qi_s[rws].rearrange("p (hp hh) n -> p hp hh n", hh=2)[:, :, hh_, :],
                      qi_d[s, rws, :].rearrange("p (hp n) -> p hp n", hp=4), r=[r_q_d[s]], w=[r_qi_s])
            P.dma("sp", wt_s.rearrange("p a n -> p (a n)"), wt_d[s], r=[r_q_d[s]], w=[r_wt_s])
            M.push()
            maskT = M.alloc(BF16, [64, 512]); r_maskT = Reg(f"maskT{s}")
            M.push()
            score = M.alloc(F32, [S]); r_score = Reg(f"score{s}")
            moff = M.off
            mrow = M.alloc(BF16, [S]); r_mrow = Reg(f"mrow{s}"); r_mrowB = Reg(f"mrowB{s}")
            cmrow = M.view(moff, F32, [2048])
            rls = [(M.alloc(F32, [512]), Reg(f"rl{s}_{i}")) for i in range(4)]
            nkg = 4 * (s + 1)
            r_sc = [Reg(f"score{s}_{i}") for i in range(nkg)]
            for qt in range(4):
                n = 0
                for hp_, kgi, hh in [(a_, b_, c_) for a_ in range(4) for b_ in range(nkg) for c_ in range(2)]:
                    h = hp_ * 2 + hh
                    if True:
                        cols = slice(kgi * 512, kgi * 512 + 512)
                        b = nbank()
                        rl, r_rl = rls[n % 4]; n += 1
                        P.op("pe", lambda e, b=b, hp_=hp_, hh=hh, cols=cols, qt=qt: e.matmul(
                            banks[b], lhsT=qi_s[:, hp_ * 2 + hh, qt * 128:(qt + 1) * 128],
                            rhs=kiT2[:, cols], start=True, stop=True),
                            r=[r_qi_s, r_kiT2], w=[r_bank[b]])
                        P.op("act", lambda e, b=b, rl=rl: e.activation(out=rl, in_=banks[b], func=AF.Relu),
                             r=[r_bank[b]], w=[r_rl])
                        if h == 0:
                            P.op("dve", lambda e, rl=rl, cols=cols, qt=qt: e.tensor_scalar(
                                out=score[:, cols], in0=rl, scalar1=wt_s[:, qt, 0:1], scalar2=None,
                                op0=ALU.mult), r=[r_rl, r_wt_s], w=[r_sc[kgi]])
                        else:
                            P.op("dve", lambda e, rl=rl, cols=cols, qt=qt, h=h: e.scalar_tensor_tensor(
                                out=score[:, cols], in0=rl, scalar=wt_s[:, qt, h:h + 1], in1=score[:, cols],
                                op0=ALU.mult, op1=ALU.add), r=[r_rl, r_wt_s, r_sc[kgi]], w=[r_sc[kgi]])
                r_score_l = r_sc
                P.op("dve", lambda e: e.tensor_reduce(out=bis[:, 0:1], in_=score[:, 0:ext], axis=AX.X, op=ALU.max,
                                                      apply_absolute_value=True), r=r_sc, w=[r_bis])
                P.op("dve", lambda e: e.tensor_scalar(out=bis[:, 5:6], in0=bis[:, 0:1], scalar1=1.001, scalar2=1e-6,
                                                      op0=ALU.mult, op1=ALU.add), r=[r_bis], w=[r_bis])
                P.op("dve", lambda e: e.tensor_scalar(out=bis[:, 8:8 + NITER], in0=pw2, scalar1=bis[:, 5:6], scalar2=None,
                                                      op0=ALU.mult), r=[r_bis, r_pw2], w=[r_bis])
                P.op("dve", lambda e: e.memset(bis[:, 2:3], 0.0), r=[r_bis], w=[r_bis])
                P.dma("sp", cmrow, cmask[qt], w=[r_mrow, r_mrowB])
                P.op("dve", lambda e: e.tensor_tensor(out=score[:, s * 2048:(s + 1) * 2048],
                                                      in0=score[:, s * 2048:(s + 1) * 2048], in1=cmrow, op=ALU.add),
                     r=r_sc[4 * s:] + [r_mrow], w=r_sc[4 * s:])
                cA = (ext * 27 // 64) // 64 * 64
                nB = ext - cA
                for k in range(NITER):
                    P.op("dve", lambda e: e.tensor_scalar(out=mrow[:, 0:cA], in0=score[:, 0:cA], scalar1=bis[:, 2:3],
                                                          scalar2=0.0, op0=ALU.is_ge, op1=ALU.add, accum_out=bisA[:, 0:1]),
                         r=r_sc + [r_bis], w=[r_mrow, r_bisA])
                    P.op("act", lambda e: e.activation(out=mrow[:, cA:ext], in_=score[:, cA:ext], func=AF.Sign,
                                                      scale=-1.0, bias=bis[:, 2:3], accum_out=bisB[:, 0:1]),
                         r=r_sc + [r_bis], w=[r_mrowB, r_bisB])
                    P.op("dve", lambda e: e.scalar_tensor_tensor(
                        out=bis[:, 7:8], in0=bisB[:, 0:1], scalar=-0.5, in1=bisA[:, 0:1], op0=ALU.mult, op1=ALU.add),
                        r=[r_bisA, r_bisB], w=[r_bis])
                    P.op("dve", lambda e: e.tensor_scalar(out=bis[:, 4:5], in0=bis[:, 7:8], scalar1=TOPK - 0.5 - nB / 2.0,
                                                          scalar2=-0.5, op0=ALU.is_ge, op1=ALU.add),
                         r=[r_bis], w=[r_bis])
                    P.op("dve", lambda e, k=k: e.scalar_tensor_tensor(
                        out=bis[:, 2:3], in0=bis[:, 4:5], scalar=bis[:, 8 + k:9 + k], in1=bis[:, 2:3],
                        op0=ALU.mult, op1=ALU.add), r=[r_bis], w=[r_bis])
                P.op("dve", lambda e: e.scalar_tensor_tensor(
                    out=bis[:, 1:2], in0=bis[:, 8 + NITER - 1:8 + NITER], scalar=-0.5, in1=bis[:, 2:3],
                    op0=ALU.mult, op1=ALU.add), r=[r_bis], w=[r_bis])
                P.op("dve", lambda e: e.tensor_scalar(out=mrow[:, 0:ext], in0=score[:, 0:ext], scalar1=bis[:, 1:2],
                                                      scalar2=None, op0=ALU.is_ge), r=r_sc + [r_bis], w=[r_mrow, r_mrowB])
                if debug:
                    P.dma("sp", thr_d[s * 4 + qt], bis[:, 0:2], r=[r_bis], w=[r_dbg])
                for g in range(nkt // 8):
                    b = nbank()
                    tp = banks[b].bitcast(BF16)
                    for j in range(8):
                        kt = g * 8 + j
                        P.op("pe", lambda e, tp=tp, j=j, kt=kt: e.transpose(
                            out=tp[:, j * 128:(j + 1) * 128], in_=mrow[:, kt * 128:(kt + 1) * 128], identity=ident),
                            r=[r_mrow, r_mrowB, r_ident], w=[r_bank[b]])
                    evac("act" if g % 2 == 0 else "dve", maskT[:, g * 8:(g + 1) * 8, qt * 128:(qt + 1) * 128],
                         tp.rearrange("p (j k) -> p j k", j=8), [r_bank[b]], [r_maskT])
            M.pop()
            P.barrier()
            if upto == "I1":
                P.dma("sp", kbT_d[0, :, 0:8192], maskT[:, 0:16, :].rearrange("p a n -> p (a n)"), r=[r_maskT], w=[r_dbg])
                P.emit()
                return True
            M.push()
            kbufs = [(M.alloc(BF16, [2, 2048]), Reg(f"kbuf{s}_{i}")) for i in range(2)]
            vbufs = [(M.alloc(BF16, [16, 256]), Reg(f"vbuf{s}_{i}")) for i in range(2)]
            pts = [(M.alloc(BF16, [512]), Reg(f"pt{s}_{i}")) for i in range(8)]
            cmT = M.alloc(BF16, [16, 512]); r_cmT = Reg(f"cmT{s}")
            P.dma("pool", cmT, cmaskT, w=[r_cmT])
            qb_s = M.alloc(BF16, [8, 512]); r_qb_s = Reg(f"qb_s{s}")
            qa_s = M.alloc(BF16, [8, 512]); r_qa_s = Reg(f"qa_s{s}")
            P.op("pool", lambda e: e.memset(qb_s, 0.0), w=[r_qb_s])
            P.op("pool", lambda e: e.memset(qa_s[96:128], 0.0), w=[r_qa_s])
            for hh_ in range(2):
                rws = slice(hh_ * 64, (hh_ + 1) * 64)
                P.dma("sp", qb_s[rws].rearrange("p (hp hh) n -> p hp hh n", hh=2)[:, :, hh_, :],
                      qb_d[s, rws, :].rearrange("p (hp n) -> p hp n", hp=4), r=[r_q_d[s]], w=[r_qb_s])
                P.dma("sp", qa_s[0:64].rearrange("p (hp hh) n -> p hp hh n", hh=2)[:, :, hh_, :],
                      qn_d[s, rws, :].rearrange("p (hp n) -> p hp n", hp=4), r=[r_q_d[s]], w=[r_qa_s])
            P.dma("sp", qa_s[64:96].rearrange("p a n -> p (a n)"), qpe_d[s], r=[r_q_d[s]], w=[r_qa_s])
            for hp in range(0 if _os.environ.get("KSKIPDSA") else 4):
                attention(s, hp, kbT_d, r_kbT_d, vb_d, r_vb_d, qb_s, r_qb_s, o_b, r_o_b, False, maskT, r_maskT,
                          kbufs, vbufs, pts, cmT, r_cmT)
            for hp in range(0 if _os.environ.get("KSKIPMLA") else 4):
                attention(s, hp, knT_d, r_knT_d, va_d, r_va_d, qa_s, r_qa_s, o_a, r_o_a, True, maskT, r_maskT,
                          kbufs, vbufs, pts, cmT, r_cmT)
            M.pop()
            M.pop()
            P.barrier()
            if debug:
                P.dma("sp", ob_d[s], o_b.rearrange("p a n -> p (a n)"), r=[r_o_b], w=[r_dbg])
                P.dma("sp", oa_d[s], o_a.rearrange("p a n -> p (a n)"), r=[r_o_a], w=[r_dbg])
            if upto == "T1":
                P.emit()
                return True

            rr["pool"] = list(range(8))
            M.push()
            Woa = M.alloc(BF16, [4, 1024]); r_Woa = Reg(f"Woa{s}")
            Wob = M.alloc(BF16, [4, 1024]); r_Wob = Reg(f"Wob{s}")
            Wout = M.alloc(BF16, [8, 1024]); r_Wout = Reg(f"Wout{s}")
            P.dma("pool", Woa, w_o_a.rearrange("(c p) n -> p c n", p=128), w=[r_Woa])
            P.dma("pool", Wob, w_o_b.rearrange("(c p) n -> p c n", p=128), w=[r_Wob])
            for c0 in range(0, 8, 4):
                P.dma("pool", Wout[:, c0:c0 + 4, :], w_out.rearrange("(c p) n -> p c n", p=128)[:, c0:c0 + 4, :], w=[r_Wout])
            ga_s = M.alloc(BF16, [8, 512]); r_ga_s = Reg(f"ga_s{s}")
            gb_s = M.alloc(BF16, [8, 512]); r_gb_s = Reg(f"gb_s{s}")
            P.dma("sp", ga_s.rearrange("p a n -> p (a n)"), ga_d[s], r=[r_q_d[s]], w=[r_ga_s])
            P.dma("sp", gb_s.rearrange("p a n -> p (a n)"), gb_d[s], r=[r_q_d[s]], w=[r_gb_s])
            yT = M.alloc(BF16, [8, 512]); r_yT = Reg(f"yT{s}")
            ft1 = [(M.alloc(F32, [512]), Reg(f"ft1_{s}_{i}")) for i in range(2)]
            ft2 = [(M.alloc(F32, [512]), Reg(f"ft2_{s}_{i}")) for i in range(2)]
            fxt = [(M.alloc(F32, [D]), Reg(f"fxt{s}_{i}")) for i in range(2)]
            fx1 = [(M.alloc(F32, [D]), Reg(f"fx1{s}_{i}")) for i in range(2)]
            for dch in range(8):
                ba = nbank()
                for hp in range(4):
                    P.op("pe", lambda e, ba=ba, hp=hp, dch=dch: e.matmul(
                        banks[ba], lhsT=Woa[:, hp, dch * 128:(dch + 1) * 128], rhs=o_a[:, hp, :],
                        start=(hp == 0), stop=(hp == 3)), r=[r_Woa, r_o_a], w=[r_bank[ba]])
                bb = nbank()
                for hp in range(4):
                    P.op("pe", lambda e, bb=bb, hp=hp, dch=dch: e.matmul(
                        banks[bb], lhsT=Wob[:, hp, dch * 128:(dch + 1) * 128], rhs=o_b[:, hp, :],
                        start=(hp == 0), stop=(hp == 3)), r=[r_Wob, r_o_b], w=[r_bank[bb]])
                t1, r_t1 = ft1[dch % 2]; t2, r_t2 = ft2[dch % 2]
                P.op("dve", lambda e, ba=ba, dch=dch, t1=t1: e.tensor_tensor(out=t1, in0=banks[ba], in1=ga_s[:, dch, :], op=ALU.mult),
                     r=[r_bank[ba], r_ga_s], w=[r_t1])
                P.op("dve", lambda e, bb=bb, dch=dch, t2=t2: e.tensor_tensor(out=t2, in0=banks[bb], in1=gb_s[:, dch, :], op=ALU.mult),
                     r=[r_bank[bb], r_gb_s], w=[r_t2])
                P.op("pool", lambda e, dch=dch, t1=t1, t2=t2: e.tensor_tensor(out=yT[:, dch, :], in0=t1, in1=t2, op=ALU.add),
                     r=[r_t1, r_t2], w=[r_yT])
            for t in range(4):
                xt, r_xt = fxt[t % 2]; x1t, r_x1t = fx1[t % 2]
                P.dma("sp", xt, xq_v[s * 4 + t], w=[r_xt])
                for half in range(2):
                    b = nbank()
                    for dch in range(8):
                        P.op("pe", lambda e, b=b, dch=dch, t=t, half=half: e.matmul(
                            banks[b], lhsT=yT[:, dch, t * 128:(t + 1) * 128], rhs=Wout[:, dch, half * 512:(half + 1) * 512],
                            start=(dch == 0), stop=(dch == 7)), r=[r_yT, r_Wout], w=[r_bank[b]])
                    P.op("dve", lambda e, b=b, half=half, xt=xt, x1t=x1t: e.tensor_tensor(
                        out=x1t[:, half * 512:(half + 1) * 512], in0=banks[b], in1=xt[:, half * 512:(half + 1) * 512],
                        op=ALU.add), r=[r_bank[b], r_xt], w=[r_x1t])
                P.dma("sp", x1_d[s * 512 + t * 128:s * 512 + (t + 1) * 128, :], x1t, r=[r_x1t], w=[r_x1_d])
            M.pop()
            P.barrier()
            rr["pool"] = [0, 1, 2, 3, 4, 5]
            return False

        for s_ in range(nslots):
            if do_slot(s_):
                return nc
        M.pop()
        P.barrier()
        rr["pool"] = list(range(8))
        if upto in ("F", "F1"):
            P.emit()
            return nc

        M.push()
        gffn_b = M.alloc(F32, [D]); r_gffn = Reg("gffn")
        gfin_b = M.alloc(F32, [D]); r_gfin = Reg("gfin")
        P.dma("sp", gffn_b, g_ffn.partition_broadcast(128), w=[r_gffn])
        P.dma("sp", gfin_b, g_fin.partition_broadcast(128), w=[r_gfin])
        Wr = M.alloc(F32, [8, 40]); r_Wr = Reg("Wr")
        for c in range(8):
            P.dma("sp", Wr[:, c, 0:8], w_rg[c * 128:(c + 1) * 128, :], w=[r_Wr])
            P.dma("sp", Wr[:, c, 8:40], w_re[c * 128:(c + 1) * 128, :], w=[r_Wr])
        brb = M.alloc(F32, [40]); r_brb = Reg("brb")
        P.dma("sp", brb[:, 0:8], b_rg.partition_broadcast(128), w=[r_brb])
        P.dma("sp", brb[:, 8:40], b_re.partition_broadcast(128), w=[r_brb])
        h2T = M.alloc(BF16, [8, NQ]); r_h2T = [Reg(f"h2T{i}") for i in range(4)]
        acc = M.alloc(F32, [16, D]); r_acc = [[Reg(f"acc{i}_{h}") for h in range(2)] for i in range(16)]
        comb = M.alloc(F32, [16, 32]); r_comb = [Reg(f"comb{i}") for i in range(16)]
        if debug:
            P.op("pool", lambda e: e.memset(comb, 0.0), w=r_comb)
        mx = []
        for i in range(2):
            mx.append(dict(xt=M.alloc(F32, [D]), r_xt=Reg(f"mxt{i}"), xn=M.alloc(BF16, [D]), r_xn=Reg(f"mxn{i}"),
                           xf=M.alloc(F32, [D]), r_xf=Reg(f"mxf{i}"), st=M.alloc(F32, [4]), r_st=Reg(f"mst{i}"),
                           jk=M.alloc(BF16, [D]), r_jk=Reg(f"mjk{i}"), hf=M.alloc(F32, [8, 128]), r_hf=Reg(f"mhf{i}"),
                           lg=M.alloc(F32, [40]), msk=M.alloc(F32, [32]), sm=M.alloc(F32, [48]), r_sm=Reg(f"msm{i}")))
        x1_v = x1_d.rearrange("(n p) d -> n p d", p=128)
        ntile = 16 if not _os.environ.get("KMOET") else int(_os.environ["KMOET"])
        nexp = 32 if not _os.environ.get("KMOEE") else int(_os.environ["KMOEE"])
        if _os.environ.get("KMOEONLY"):
            for t in range(16):
                P.dma("sp", x1_v[t], xq_v[t], w=[r_x1_d])
        def moe_stageA(t):
            m = mx[t % 2]
            xt, r_xt, xn, r_xn, xf, r_xf, st, r_st, jk, r_jk = (m["xt"], m["r_xt"], m["xn"], m["r_xn"], m["xf"], m["r_xf"],
                                                              m["st"], m["r_st"], m["jk"], m["r_jk"])
            hf, r_hf, lg, msk, sm, r_sm = m["hf"], m["r_hf"], m["lg"], m["msk"], m["sm"], m["r_sm"]
            P.dma("sp", xt, x1_v[t], r=[r_x1_d], w=[r_xt])
            P.op("act", lambda e, jk=jk, xt=xt, st=st: e.activation(out=jk, in_=xt, func=AF.Square, accum_out=st[:, 0:1]),
                 r=[r_xt], w=[r_jk, r_st])
            P.op("act", lambda e, st=st: e.activation(out=st[:, 1:2], in_=st[:, 0:1], func=AF.Sqrt, scale=1.0 / D, bias=EPS),
                 r=[r_st], w=[r_st])
            P.op("dve", lambda e, st=st: e.reciprocal(out=st[:, 2:3], in_=st[:, 1:2]), r=[r_st], w=[r_st])
            P.op("dve", lambda e, xf=xf, xt=xt, st=st: e.scalar_tensor_tensor(
                out=xf, in0=xt, scalar=st[:, 2:3], in1=gffn_b, op0=ALU.mult, op1=ALU.mult),
                r=[r_xt, r_st, r_gffn], w=[r_xf])
            P.op("pool", lambda e, xn=xn, xf=xf: e.tensor_copy(out=xn, in_=xf), r=[r_xf], w=[r_xn])
            b = nbank()
            tp = banks[b].bitcast(BF16)
            for c in range(8):
                P.op("pe", lambda e, tp=tp, xn=xn, c=c: e.transpose(
                    out=tp[:, c * 128:(c + 1) * 128], in_=xn[:, c * 128:(c + 1) * 128], identity=ident),
                    r=[r_xn, r_ident], w=[r_bank[b]])
            evac("act", h2T[:, :, t * 128:(t + 1) * 128], tp.rearrange("p (c k) -> p c k", c=8), [r_bank[b]],
                 [r_h2T[t // 4]])
            for half in range(2):
                b = nbank()
                for c4 in range(4):
                    c = half * 4 + c4
                    P.op("pe", lambda e, b=b, c=c, c4=c4, xf=xf: e.transpose(
                        out=banks[b][:, c4 * 128:(c4 + 1) * 128], in_=xf[:, c * 128:(c + 1) * 128], identity=ident_f),
                        r=[r_xf, r_identf], w=[r_bank[b]])
                evac("dve", hf[:, half * 4:(half + 1) * 4, :], banks[b].rearrange("p (c k) -> p c k", c=4),
                     [r_bank[b]], [r_hf])
            b = nbank()
            for c in range(8):
                P.op("pe", lambda e, b=b, c=c, hf=hf: e.matmul(banks[b][:, 0:40], lhsT=hf[:, c, :], rhs=Wr[:, c, :],
                                                             start=(c == 0), stop=(c == 7)),
                     r=[r_hf, r_Wr], w=[r_bank[b]])
            return b

        def moe_stageB(t, b):
            m = mx[t % 2]
            lg, msk, sm, r_sm = m["lg"], m["msk"], m["sm"], m["r_sm"]
            rsm = [r_sm]
            P.op("dve", lambda e, b=b, lg=lg: e.tensor_tensor(out=lg, in0=banks[b][:, 0:40], in1=brb, op=ALU.add),
                 r=[r_bank[b], r_brb], w=rsm)
            P.op("dve", lambda e, lg=lg, sm=sm: e.tensor_reduce(out=sm[:, 0:1], in_=lg[:, 0:8], axis=AX.X, op=ALU.max),
                 r=rsm, w=rsm)
            P.op("dve", lambda e, sm=sm: e.tensor_scalar(out=sm[:, 1:2], in0=sm[:, 0:1], scalar1=-1.0, scalar2=None,
                                                         op0=ALU.mult), r=rsm, w=rsm)
            P.op("act", lambda e, lg=lg, sm=sm: e.activation(out=sm[:, 8:16], in_=lg[:, 0:8], func=AF.Exp, bias=sm[:, 1:2],
                                                            scale=1.0, accum_out=sm[:, 2:3]), r=rsm, w=rsm)
            P.op("dve", lambda e, sm=sm: e.reciprocal(out=sm[:, 3:4], in_=sm[:, 2:3]), r=rsm, w=rsm)
            P.op("dve", lambda e, lg=lg, sm=sm: e.tensor_scalar(out=sm[:, 16:24], in0=lg[:, 0:8], scalar1=sm[:, 0:1],
                                                               scalar2=None, op0=ALU.is_equal), r=rsm, w=rsm)
            P.op("dve", lambda e, sm=sm: e.tensor_scalar(out=sm[:, 24:32], in0=sm[:, 16:24], scalar1=-1.0, scalar2=1.0e30,
                                                         op0=ALU.add, op1=ALU.mult), r=rsm, w=rsm)
            P.op("dve", lambda e, lg=lg, sm=sm, msk=msk: e.tensor_tensor(
                out=msk.rearrange("p (g k) -> p g k", k=4), in0=lg[:, 8:40].rearrange("p (g k) -> p g k", k=4),
                in1=sm[:, 24:32].rearrange("p (g o) -> p g o", o=1).to_broadcast([128, 8, 4]), op=ALU.add),
                r=rsm, w=rsm)
            P.op("dve", lambda e, sm=sm, msk=msk: e.max(out=sm[:, 32:40], in_=msk), r=rsm, w=rsm)
            P.op("dve", lambda e, sm=sm: e.tensor_scalar(out=sm[:, 4:5], in0=sm[:, 32:33], scalar1=-1.0, scalar2=None,
                                                         op0=ALU.mult), r=rsm, w=rsm)
            P.op("act", lambda e, sm=sm: e.activation(out=sm[:, 5:6], in_=sm[:, 33:34], func=AF.Exp, bias=sm[:, 4:5],
                                                     scale=1.0), r=rsm, w=rsm)
            P.op("dve", lambda e, sm=sm: e.tensor_scalar(out=sm[:, 6:7], in0=sm[:, 5:6], scalar1=1.0, scalar2=None,
                                                         op0=ALU.add), r=rsm, w=rsm)
            P.op("dve", lambda e, sm=sm: e.reciprocal(out=sm[:, 6:7], in_=sm[:, 6:7]), r=rsm, w=rsm)
            P.op("dve", lambda e, sm=sm: e.tensor_tensor(out=sm[:, 6:7], in0=sm[:, 6:7], in1=sm[:, 3:4], op=ALU.mult),
                 r=rsm, w=rsm)
            P.op("dve", lambda e, sm=sm: e.tensor_tensor(out=sm[:, 7:8], in0=sm[:, 6:7], in1=sm[:, 5:6], op=ALU.mult),
                 r=rsm, w=rsm)
            P.op("dve", lambda e, sm=sm, msk=msk, lg=lg: e.tensor_scalar(
                out=lg[:, 8:40], in0=msk, scalar1=sm[:, 32:33], scalar2=sm[:, 6:7], op0=ALU.is_equal, op1=ALU.mult),
                r=rsm, w=rsm)
            P.op("dve", lambda e, sm=sm, msk=msk: e.tensor_scalar(
                out=msk, in0=msk, scalar1=sm[:, 33:34], scalar2=sm[:, 7:8], op0=ALU.is_equal, op1=ALU.mult),
                r=rsm, w=rsm)
            P.op("dve", lambda e, t=t, msk=msk, lg=lg: e.tensor_tensor(out=comb[:, t, :], in0=lg[:, 8:40], in1=msk, op=ALU.add),
                 r=rsm, w=[r_comb[t]])

        prevb = None
        for t in range(ntile):
            bb_ = moe_stageA(t)
            if prevb is not None:
                moe_stageB(t - 1, prevb)
            prevb = bb_
        if prevb is not None:
            moe_stageB(ntile - 1, prevb)
        if debug:
            P.dma("sp", comb_d, comb.rearrange("p a n -> p (a n)"), r=r_comb, w=[r_dbg])
        wgs = [(M.alloc(BF16, [8, 256]), Reg(f"wg{i}")) for i in range(2)]
        wus = [(M.alloc(BF16, [8, 256]), Reg(f"wu{i}")) for i in range(2)]
        wds = [(M.alloc(BF16, [2, D]), Reg(f"wd{i}")) for i in range(2)]
        wdf = [(M.alloc(F32, [2, D]), Reg(f"wdf{i}")) for i in range(2)]
        sgs = [(M.alloc(F32, [512]), Reg(f"sg{i}")) for i in range(2)]
        hids = [(M.alloc(BF16, [2, 512]), Reg(f"hid{i}")) for i in range(2)]
        tms = [(M.alloc(F32, [512]), Reg(f"tm{i}")) for i in range(2)]
        k = 0
        for ex in range(nexp):
            Wg_e, r_Wg = wgs[ex % 2]; Wu_e, r_Wu = wus[ex % 2]; Wd_e, r_Wd = wds[ex % 2]
            if ex < 2 or not _os.environ.get("KMOENODMA"):
                P.dma("pool", Wg_e, w_gate[ex].rearrange("(c p) f -> p c f", p=128), w=[r_Wg])
                P.dma("pool", Wu_e, w_up[ex].rearrange("(c p) f -> p c f", p=128), w=[r_Wu])
                Wdf_e, r_Wdf = wdf[ex % 2]
                P.dma("sp", Wdf_e, w_down[ex].rearrange("(c p) n -> p c n", p=128), w=[r_Wdf])
                P.op("pool", lambda e, Wd_e=Wd_e, Wdf_e=Wdf_e: e.tensor_copy(out=Wd_e, in_=Wdf_e), r=[r_Wdf], w=[r_Wd])
            for blk in range((ntile + 3) // 4):
                hid, r_hid = hids[k % 2]; k += 1
                for ffc in range(2):
                    bg = nbank()
                    for c in range(8):
                        P.op("pe", lambda e, bg=bg, c=c, ffc=ffc, blk=blk, Wg_e=Wg_e: e.matmul(
                            banks[bg], lhsT=Wg_e[:, c, ffc * 128:(ffc + 1) * 128], rhs=h2T[:, c, blk * 512:(blk + 1) * 512],
                            start=(c == 0), stop=(c == 7)), r=[r_Wg, r_h2T[blk]], w=[r_bank[bg]])
                    bu = nbank()
                    for c in range(8):
                        P.op("pe", lambda e, bu=bu, c=c, ffc=ffc, blk=blk, Wu_e=Wu_e: e.matmul(
                            banks[bu], lhsT=Wu_e[:, c, ffc * 128:(ffc + 1) * 128], rhs=h2T[:, c, blk * 512:(blk + 1) * 512],
                            start=(c == 0), stop=(c == 7)), r=[r_Wu, r_h2T[blk]], w=[r_bank[bu]])
                    sg, r_sg = sgs[ffc]
                    P.op("act", lambda e, bg=bg, sg=sg: e.activation(out=sg, in_=banks[bg], func=AF.Silu),
                         r=[r_bank[bg]], w=[r_sg])
                    P.op("dve", lambda e, bu=bu, sg=sg, hid=hid, ffc=ffc: e.tensor_tensor(
                        out=hid[:, ffc, :], in0=banks[bu], in1=sg, op=ALU.mult), r=[r_bank[bu], r_sg], w=[r_hid])
                for t4 in range(4):
                    t = blk * 4 + t4
                    if t >= ntile:
                        continue
                    for half in range(2):
                        bd = nbank()
                        for ffc in range(2):
                            P.op("pe", lambda e, bd=bd, ffc=ffc, t4=t4, half=half, hid=hid, Wd_e=Wd_e: e.matmul(
                                banks[bd], lhsT=hid[:, ffc, t4 * 128:(t4 + 1) * 128],
                                rhs=Wd_e[:, ffc, half * 512:(half + 1) * 512], start=(ffc == 0), stop=(ffc == 1)),
                                r=[r_hid, r_Wd], w=[r_bank[bd]])
                        a_ = acc[:, t, half * 512:(half + 1) * 512]
                        cw = comb[:, t, ex:ex + 1]
                        if ex == 0:
                            P.op("dve", lambda e, bd=bd, a_=a_, cw=cw: e.tensor_scalar(
                                out=a_, in0=banks[bd], scalar1=cw, scalar2=None, op0=ALU.mult),
                                r=[r_bank[bd], r_comb[t]], w=[r_acc[t][half]])
                        elif half == 0:
                            P.op("dve", lambda e, bd=bd, a_=a_, cw=cw: e.scalar_tensor_tensor(
                                out=a_, in0=banks[bd], scalar=cw, in1=a_, op0=ALU.mult, op1=ALU.add),
                                r=[r_bank[bd], r_comb[t], r_acc[t][half]], w=[r_acc[t][half]])
                        else:
                            tm, r_tm = tms[t4 % 2]
                            P.op("act", lambda e, bd=bd, tm=tm, cw=cw: e.activation(
                                out=tm, in_=banks[bd], func=AF.Copy, scale=cw), r=[r_bank[bd], r_comb[t]], w=[r_tm])
                            P.op("pool", lambda e, a_=a_, tm=tm: e.tensor_tensor(out=a_, in0=a_, in1=tm, op=ALU.add),
                                 r=[r_tm, r_acc[t][half]], w=[r_acc[t][half]])
        for t in range(ntile):
            m = mx[t % 2]
            xt, r_xt, xf, r_xf, st, r_st, jk, r_jk = m["xt"], m["r_xt"], m["xf"], m["r_xf"], m["st"], m["r_st"], m["jk"], m["r_jk"]
            P.dma("sp", xt, x1_v[t], r=[r_x1_d], w=[r_xt])
            P.op("pool", lambda e, xt=xt, t=t: e.tensor_tensor(out=xt, in0=xt, in1=acc[:, t, :], op=ALU.add),
                 r=[r_xt] + r_acc[t], w=[r_xt])
            P.op("act", lambda e, jk=jk, xt=xt, st=st: e.activation(out=jk, in_=xt, func=AF.Square, accum_out=st[:, 0:1]),
                 r=[r_xt], w=[r_jk, r_st])
            P.op("act", lambda e, st=st: e.activation(out=st[:, 1:2], in_=st[:, 0:1], func=AF.Sqrt, scale=1.0 / D, bias=EPS),
                 r=[r_st], w=[r_st])
            P.op("dve", lambda e, st=st: e.reciprocal(out=st[:, 2:3], in_=st[:, 1:2]), r=[r_st], w=[r_st])
            P.op("dve", lambda e, xf=xf, xt=xt, st=st: e.scalar_tensor_tensor(
                out=xf, in0=xt, scalar=st[:, 2:3], in1=gfin_b, op0=ALU.mult, op1=ALU.mult),
                r=[r_xt, r_st, r_gfin], w=[r_xf])
            P.dma("sp", out[t * 128:(t + 1) * 128, :], xf, r=[r_xf])
        M.pop()

        P.emit()
    return nc


def perm_matrix(block, half):
    pm = np.zeros((128, 128), np.float32)
    for m in range(128):
        j = m % block
        if j < half:
            pm[m + half, m] = 1.0
        elif j < 2 * half:
            pm[m - half, m] = 1.0
    return pm


def nc_input_names(nc):
    return list(getattr(nc, "_in_names"))


def make_in_maps(inputs):
    x = np.ascontiguousarray(inputs["x"], dtype=np.float32)
    cs16, cs32 = rope_tables()
    shared = {
        "w_in": inputs["w_in"][0], "w_uq": inputs["w_uq"][0], "w_uk": inputs["w_uk"][0],
        "w_uv": inputs["w_uv"][0], "w_o_a": inputs["w_o_a"][0], "w_o_b": inputs["w_o_b"][0],
        "w_out": inputs["w_out"][0], "w_rg": inputs["w_router_group"][0], "w_re": inputs["w_router_expert"][0],
        "b_rg": inputs["b_router_group"][0], "b_re": inputs["b_router_expert"][0],
        "w_gate": inputs["w_gate"][0], "w_up": inputs["w_up"][0], "w_down": inputs["w_down"][0],
        "g_mix": inputs["norm_mix_g"][0], "g_q": inputs["mla_q_norm_g"][0], "g_kv": inputs["mla_kv_norm_g"][0],
        "g_ffn": inputs["norm_ffn_g"][0], "g_fin": inputs["final_norm_g"],
        "cs16k": cs16, "cs32k": cs32, "pm16": perm_matrix(64, 8), "pm32": perm_matrix(32, 16),
    }
    shared = {k: np.ascontiguousarray(v, dtype=np.float32) for k, v in shared.items()}
    in_maps = []
    for c in range(8):
        b, r = divmod(c, 4)
        rows = np.concatenate([np.arange((4 * s + r) * 512, (4 * s + r + 1) * 512) for s in range(4)])
        m = dict(shared)
        m["xb"] = x[b]
        m["xq"] = np.ascontiguousarray(x[b][rows])
        m["cs16q"] = np.ascontiguousarray(cs16[:, :, rows])
        m["cs32q"] = np.ascontiguousarray(cs32[:, :, rows])
        m["cs32q4"] = np.ascontiguousarray(np.tile(cs32[:, :, rows], (1, 4, 1)))
        qpos = 512 * r + np.arange(512)
        kpos = np.arange(2048)
        adm = (kpos[None, :] // 64) <= (qpos[:, None] // 64)
        m["cmask"] = np.where(adm, 0.0, NEGM).astype(np.float32).reshape(4, 128, 2048)
        m["cmaskT"] = np.ascontiguousarray(
            adm.T.astype(np.float32).reshape(16, 128, 512).transpose(1, 0, 2))
        in_maps.append(m)
    return in_maps


def kernel(**inputs):
    nc = build()
    in_maps = make_in_maps(inputs)
    names = set(nc_input_names(nc))
    in_maps = [{k: v for k, v in m.items() if k in names} for m in in_maps]
    res = run_bass_kernel_spmd(nc, in_maps, core_ids=list(range(8)))
    outp = np.zeros((2, S, D), np.float32)
    for c in range(8):
        b, r = divmod(c, 4)
        o = res.results[c]["out"]
        for s in range(4):
            g = 4 * s + r
            outp[b, g * 512:(g + 1) * 512] = o[s * 512:(s + 1) * 512]
    return outp
```

```python
import numpy as np
from contextlib import ExitStack
import concourse.bass as bass
import concourse.mybir as mybir
from concourse.bass_utils import run_bass_kernel_spmd

F32 = mybir.dt.float32
BF16 = mybir.dt.bfloat16
AF = mybir.ActivationFunctionType
ALU = mybir.AluOpType
AX = mybir.AxisListType

S = 8192
D = 1024
NQ = 2048
EPS = 1e-6
THETA = 500000.0
import os as _os0
NITER = int(_os0.environ.get('KNITER', '14'))
TOPK = 256
NEGM = -1.0e30


class Reg:
    __slots__ = ("name", "w", "rs", "rd", "xr", "excl")

    def __init__(self, name, excl=False):
        self.name = name
        self.w = None
        self.rs = {}
        self.rd = []
        self.xr = None
        self.excl = excl


class Op:
    __slots__ = ("eng", "fn", "deps", "need_sig", "sig", "is_dma", "sem_i", "sem_v")


class Prog:
    ENGS = ("pe", "act", "dve", "pool", "sp")
    EPOCH = 8000

    def __init__(self, nc, n_dma_sems=48):
        self.nc = nc
        self.ops = {e: [] for e in self.ENGS}
        self.n_dma_sems = n_dma_sems
        self.dma_last = [None] * n_dma_sems
        self.dma_cnt = [0] * n_dma_sems
        self.dma_rr = 0
        self.dma_ranges = {"sp": (0, 28), "pool": (28, 40), "act": (40, 48)}
        self.dma_rrs = {"sp": 0, "pool": 0, "act": 0}

    def _mk(self, eng, fn, r, w, is_dma, stream):
        self.count = getattr(self, "count", 0) + 1
        if self.count > getattr(self, "limit", 10 ** 9):
            return None
        o = Op()
        o.eng = eng; o.fn = fn; o.need_sig = False; o.sig = None; o.is_dma = is_dma
        o.sem_i = None; o.sem_v = None
        deps = {}
        pb = getattr(self, "pending_barrier", None)
        if pb and pb.get(eng):
            for d in pb.pop(eng):
                if d.is_dma or d.eng != eng:
                    deps[id(d)] = d

        def add(d, raw=False):
            if d is None:
                return
            if d.is_dma or is_dma or d.eng != eng or (raw and eng != "pe" and not stream):
                deps[id(d)] = d
        for reg in r:
            add(reg.w, True)
            if reg.excl:
                add(reg.xr)
        for reg in w:
            add(reg.w)
            for d in reg.rs.values():
                add(d)
            for d in reg.rd:
                add(d)
        if is_dma:
            lo, hi = self.dma_ranges[eng]
            i = lo + self.dma_rrs[eng] % (hi - lo)
            self.dma_rrs[eng] += 1
            prev = self.dma_last[i]
            if prev is not None:
                deps[id(prev)] = prev
            self.dma_cnt[i] += 1
            o.sem_i = i; o.sem_v = 16 * self.dma_cnt[i]
            self.dma_last[i] = o
        o.deps = list(deps.values())
        for d in o.deps:
            if not d.is_dma:
                d.need_sig = True
        for reg in r:
            if is_dma:
                reg.rd.append(o)
            else:
                reg.rs[eng] = o
                if reg.excl:
                    reg.xr = o
        for reg in w:
            reg.w = o; reg.rs = {}; reg.rd = []; reg.xr = None
        self.ops[eng].append(o)
        return o

    def barrier(self):
        lasts = [self.ops[e][-1] for e in self.ENGS if self.ops[e]]
        lasts += [d for d in self.dma_last if d is not None]
        self.pending_barrier = {e: list(lasts) for e in self.ENGS}

    def op(self, eng, fn, r=(), w=(), stream=False):
        return self._mk(eng, fn, r, w, False, stream)

    def dma(self, eng, out, in_, r=(), w=()):
        return self._mk(eng, lambda e: e.dma_start(out=out, in_=in_), r, w, True, False)

    def emit(self):
        nc = self.nc
        final_deps = [d for d in self.dma_last if d is not None]
        nsig = {}
        for eng in self.ENGS:
            c = 0
            for o in self.ops[eng]:
                if o.need_sig and not o.is_dma:
                    c += 1
                    o.sig = c
            nsig[eng] = c
        with ExitStack() as st:
            eng_sems = {}
            for eng in self.ENGS:
                n = nsig[eng] // self.EPOCH + 1
                eng_sems[eng] = [st.enter_context(nc.semaphore(f"s_{eng}_{k}")) for k in range(n)]
            dma_sems = [st.enter_context(nc.semaphore(f"s_dma_{k}")) for k in range(self.n_dma_sems)]
            block = st.enter_context(nc.Block())

            def tok(d):
                if d.is_dma:
                    return dma_sems[d.sem_i], d.sem_v, ("d", d.sem_i)
                k = (d.sig - 1) // self.EPOCH
                return eng_sems[d.eng][k], d.sig - k * self.EPOCH, (d.eng, k)

            def run(eng, e):
                waited = {}
                for o in self.ops[eng]:
                    for d in o.deps:
                        sem, v, key = tok(d)
                        if waited.get(key, 0) < v:
                            e.wait_ge(sem, v)
                            waited[key] = v
                    ins = o.fn(e)
                    if o.is_dma:
                        ins.then_inc(dma_sems[o.sem_i], 16)
                    elif o.need_sig:
                        k = (o.sig - 1) // self.EPOCH
                        ins.then_inc(eng_sems[eng][k], 1)
                if eng == "sp":
                    for d in final_deps:
                        sem, v, key = tok(d)
                        if waited.get(key, 0) < v:
                            e.wait_ge(sem, v)
                            waited[key] = v

            @block.tensor
            def _(e):
                run("pe", e)

            @block.scalar
            def _(e):
                run("act", e)

            @block.vector
            def _(e):
                run("dve", e)

            @block.gpsimd
            def _(e):
                run("pool", e)

            @block.sync
            def _(e):
                run("sp", e)


ARENA_BYTES = 204 * 1024


class Mem:
    def __init__(self, arena):
        self.arena = arena
        self.off = 0
        self.marks = []

    def push(self):
        self.marks.append(self.off)

    def pop(self):
        self.off = self.marks.pop()

    def alloc(self, dt, free_shape, p0=0, p1=128, name=None):
        ap = self.view(self.off, dt, free_shape, p0, p1)
        self.off += self._nb(dt, free_shape)
        return ap

    @staticmethod
    def _nb(dt, free_shape):
        esz = 4 if dt == F32 else 2
        n = 1
        for k in free_shape:
            n *= k
        return (n * esz + 3) // 4 * 4

    def view(self, off, dt, free_shape, p0=0, p1=128):
        esz = 4 if dt == F32 else 2
        n = 1
        for k in free_shape:
            n *= k
        nb = (n * esz + 3) // 4 * 4
        assert off + nb <= ARENA_BYTES, f"SBUF arena overflow {off}+{nb}"
        o4 = off // 4
        ap = self.arena[p0:p1, o4:o4 + nb // 4]
        if dt != F32:
            ap = ap.bitcast(dt)
        if n * esz != nb:
            ap = ap[:, 0:n]
        if len(free_shape) == 2:
            ap = ap.rearrange("p (a b) -> p a b", a=free_shape[0])
        elif len(free_shape) == 3:
            ap = ap.rearrange("p (a b c) -> p a b c", a=free_shape[0], b=free_shape[1])
        return ap


def rope_tables():
    pos = np.arange(S, dtype=np.float32)
    half = 8
    inv = (THETA ** (-np.arange(half, dtype=np.float32) * 2.0 / 16)).astype(np.float32)
    ang = pos[None, :] * inv[:, None]
    c16 = np.ones((128, S), np.float32)
    s16 = np.zeros((128, S), np.float32)
    for base in (0, 64):
        c16[base:base + 8] = np.cos(ang); c16[base + 8:base + 16] = np.cos(ang)
        s16[base:base + 8] = -np.sin(ang); s16[base + 8:base + 16] = np.sin(ang)
    half = 16
    inv = (THETA ** (-np.arange(half, dtype=np.float32) * 2.0 / 32)).astype(np.float32)
    ang = pos[None, :] * inv[:, None]
    c32 = np.concatenate([np.cos(ang), np.cos(ang)], 0).astype(np.float32)
    s32 = np.concatenate([-np.sin(ang), np.sin(ang)], 0).astype(np.float32)
    return np.stack([c16, s16]), np.stack([c32, s32])


def build(debug=False, upto="all"):
    nc = bass.Bass("TRN2", target_bir_lowering=False)
    kind_dbg = "ExternalOutput" if debug else "Internal"

    in_names = []
    nc._in_names = in_names

    def din(name, shape, dt=F32):
        in_names.append(name)
        return nc.dram_tensor(name, list(shape), dt, kind="ExternalInput").ap()

    def dscr(name, shape, dt):
        if debug:
            return nc.dram_tensor(name, list(shape), dt, kind="ExternalOutput").ap()
        return nc.dram_tensor(name, list(shape), dt).ap()

    xb = din("xb", [S, D])
    xq = din("xq", [NQ, D])
    w_in = din("w_in", [D, 4840])
    w_uq = din("w_uq", [384, 768])
    w_uk = din("w_uk", [256, 512])
    w_uv = din("w_uv", [256, 512])
    w_o_a = din("w_o_a", [512, D])
    w_o_b = din("w_o_b", [512, D])
    w_out = din("w_out", [D, D])
    w_rg = din("w_rg", [D, 8])
    w_re = din("w_re", [D, 32])
    b_rg = din("b_rg", [8])
    b_re = din("b_re", [32])
    early = upto in ("W", "N", "A1", "A", "Q", "Q1", "I1", "T1", "T", "F", "F1")
    if not early:
        w_gate = din("w_gate", [32, D, 256])
        w_up = din("w_up", [32, D, 256])
        w_down = din("w_down", [32, 256, D])
    g_mix = din("g_mix", [D])
    g_q = din("g_q", [384])
    g_kv = din("g_kv", [256])
    g_ffn = din("g_ffn", [D])
    g_fin = din("g_fin", [D])
    cs16k = din("cs16k", [2, 128, S])
    cs32k = din("cs32k", [2, 32, S])
    cs16q = din("cs16q", [2, 128, NQ])
    cs32q = din("cs32q", [2, 32, NQ])
    pm16_d = din("pm16", [128, 128])
    pm32_d = din("pm32", [128, 128])
    cs32q4 = din("cs32q4", [2, 128, NQ])
    cmask = din("cmask", [4, 128, 2048])
    cmaskT = din("cmaskT", [128, 16, 512])
    out = nc.dram_tensor("out", [NQ, D], F32, kind="ExternalOutput").ap()

    kbT_d = dscr("kbT_d", [4, 128, S], BF16)
    knT_d = dscr("knT_d", [8, 128, S], BF16)
    vb_d = dscr("vb_d", [S, 1024], BF16)
    va_d = dscr("va_d", [S, 1024], BF16)
    ki_d = dscr("ki_d", [128, S], BF16)
    kpe_d = dscr("kpe_d", [32, S], BF16)
    qb_d = dscr("qb_d", [4, 128, 2048], BF16)
    qi_d = dscr("qi_d", [4, 128, 2048], BF16)
    qn_d = dscr("qn_d", [4, 128, 2048], BF16)
    qpe_d = dscr("qpe_d", [4, 32, 4096], BF16)
    wt_d = dscr("wt_d", [4, 128, 32], F32)
    ga_d = dscr("ga_d", [4, 128, 4096], BF16)
    gb_d = dscr("gb_d", [4, 128, 4096], BF16)
    x1_d = dscr("x1_d", [NQ, D], F32)
    wf_d = dscr("wf_d", [128, 16384], BF16)
    ob_d = dscr("ob_d", [4, 128, 2048], BF16) if debug else None
    oa_d = dscr("oa_d", [4, 128, 2048], BF16) if debug else None
    thr_d = dscr("thr_d", [16, 128, 2], F32) if debug else None
    comb_d = dscr("comb_d", [128, 512], F32) if debug else None
    r_kbT_d, r_knT_d, r_vb_d, r_va_d = Reg("kbT_d"), Reg("knT_d"), Reg("vb_d"), Reg("va_d")
    r_q_d = [Reg(f"q_d{s}") for s in range(4)]
    r_x1_d = Reg("x1_d")
    r_wf_d = Reg("wf_d")
    r_dbg = Reg("dbg")

    P = Prog(nc)
    import os as _os
    P.limit = int(_os.environ.get("KLIMIT", "1000000000"))
    with nc.sbuf_tensor("arena", [128, ARENA_BYTES // 4], F32) as arena, \
            nc.psum_tensor("psum", [128, 4096], F32) as psum:
        M = Mem(arena)
        banks = [psum[:, i * 512:(i + 1) * 512] for i in range(8)]
        r_bank = [Reg(f"bank{i}", excl=True) for i in range(8)]
        rr = {"i": 0, "pool": list(range(8))}

        def nbank():
            pool = rr["pool"]
            i = pool[rr["i"] % len(pool)]
            rr["i"] += 1
            return i

        ident_f = M.alloc(F32, [128]); r_identf = Reg("identf")
        ident = M.alloc(BF16, [128]); r_ident = Reg("ident")
        ones_b = M.alloc(BF16, [128]); r_ones = Reg("ones")
        gmix_b = M.alloc(F32, [D]); r_gmix = Reg("gmix")
        P.op("pool", lambda e: e.memset(ident_f, 1.0), w=[r_identf])
        P.op("pool", lambda e: e.affine_select(out=ident_f, in_=ident_f, pattern=[[-1, 128]],
                                               compare_op=ALU.is_equal, fill=0.0, base=0,
                                               channel_multiplier=1), r=[r_identf], w=[r_identf])
        P.op("pool", lambda e: e.tensor_copy(out=ident, in_=ident_f), r=[r_identf], w=[r_ident])
        P.op("pool", lambda e: e.memset(ones_b, 1.0), w=[r_ones])
        P.dma("sp", gmix_b, g_mix.partition_broadcast(128), w=[r_gmix])
        pm16 = M.alloc(BF16, [128]); r_pm16 = Reg("pm16")
        pm32 = M.alloc(BF16, [128]); r_pm32 = Reg("pm32")
        P.dma("pool", pm16, pm16_d, w=[r_pm16])
        P.dma("pool", pm32, pm32_d, w=[r_pm32])

        r_ki_d, r_kpe_d = Reg("ki_d"), Reg("kpe_d")

        def norm_transpose(src_rows, gb_ap, r_gb, bufs, hT, r_hT, blk_tag, part="both"):
            G = len(bufs)
            for g0 in range(0, 4, G):
                grp = list(range(g0, min(4, g0 + G)))
                for t in (grp if part in ("both", "stats") else []):
                    xt, r_xt, xn, r_xn, st, r_st, jk, r_jk = bufs[t % G]
                    P.dma("sp", xt, src_rows(t), w=[r_xt])
                    P.op("act", lambda e, xt=xt, jk=jk, st=st: e.activation(
                        out=jk, in_=xt, func=AF.Square, accum_out=st[:, 0:1]), r=[r_xt], w=[r_jk, r_st])
                    P.op("act", lambda e, st=st: e.activation(
                        out=st[:, 1:2], in_=st[:, 0:1], func=AF.Sqrt, scale=1.0 / D, bias=EPS),
                        r=[r_st], w=[r_st])
                for t in (grp if part in ("both", "stats") else []):
                    xt, r_xt, xn, r_xn, st, r_st, jk, r_jk = bufs[t % G]
                    P.op("dve", lambda e, st=st: e.reciprocal(out=st[:, 2:3], in_=st[:, 1:2]),
                         r=[r_st], w=[r_st])
                    P.op("dve", lambda e, xn=xn, xt=xt, st=st: e.scalar_tensor_tensor(
                        out=xn, in0=xt, scalar=st[:, 2:3], in1=gb_ap, op0=ALU.mult, op1=ALU.mult),
                        r=[r_xt, r_st, r_gb], w=[r_xn])
                pend = []
                for t in (grp if part in ("both", "T") else []):
                    xt, r_xt, xn, r_xn, st, r_st, jk, r_jk = bufs[t % G]
                    b = nbank()
                    tp = banks[b].bitcast(BF16)
                    for c in range(8):
                        P.op("pe", lambda e, tp=tp, xn=xn, c=c: e.transpose(
                            out=tp[:, c * 128:(c + 1) * 128], in_=xn[:, c * 128:(c + 1) * 128],
                            identity=ident), r=[r_xn, r_ident], w=[r_bank[b]])
                    pend.append((t, b, tp))
                for (t, b, tp) in pend:
                    src = tp.rearrange("p (c k) -> p c k", c=8)
                    dst = hT[:, :, t * 128:(t + 1) * 128]
                    evac("act" if t % 2 == 0 else "dve", dst, src, [r_bank[b]], [r_hT])

        def mk_xbufs(n=2):
            bufs = []
            for i in range(n):
                xt = M.alloc(F32, [D]); xn = M.alloc(BF16, [D]); st = M.alloc(F32, [4])
                jk = M.alloc(BF16, [D])
                bufs.append((xt, Reg(f"xt{i}"), xn, Reg(f"xn{i}"), st, Reg(f"st{i}"), jk, Reg(f"jk{i}")))
            return bufs

        def evac(eng, dst, src, r, w, scale=None, func=None):
            if eng == "act":
                if scale is None:
                    P.op("act", lambda e: e.activation(out=dst, in_=src, func=func or AF.Copy), r=r, w=w)
                else:
                    P.op("act", lambda e: e.activation(out=dst, in_=src, func=func or AF.Copy, scale=scale),
                         r=r, w=w)
            else:
                if scale is None:
                    P.op("dve", lambda e: e.tensor_copy(out=dst, in_=src), r=r, w=w)
                else:
                    P.op("dve", lambda e: e.tensor_scalar(out=dst, in0=src, scalar1=scale, scalar2=None,
                                                          op0=ALU.mult), r=r, w=w)

        def proj_T(wt, r_wt, col0, m, hT, r_hT, nchunk, p1=None):
            b = nbank()
            o = banks[b][0:m, :]
            for c in range(nchunk):
                P.op("pe", lambda e, o=o, c=c: e.matmul(
                    o, lhsT=wt[:, c, col0:col0 + m], rhs=hT[:, c, :], start=(c == 0), stop=(c == nchunk - 1)),
                    r=[r_wt, r_hT], w=[r_bank[b]])
            return b

        def rope_combine(b0, b1, m, cos, sin, r_cs, dst, r_dst, t1, r_t1, t2, r_t2, scale=None):
            a = banks[b0][0:m, :]; bb = banks[b1][0:m, :]
            if scale is None:
                P.op("dve", lambda e: e.tensor_tensor(out=t1, in0=a, in1=cos, op=ALU.mult),
                     r=[r_bank[b0], r_cs], w=[r_t1])
                P.op("dve", lambda e: e.tensor_tensor(out=t2, in0=bb, in1=sin, op=ALU.mult),
                     r=[r_bank[b1], r_cs], w=[r_t2])
            else:
                P.op("dve", lambda e: e.scalar_tensor_tensor(out=t1, in0=a, scalar=scale, in1=cos,
                                                             op0=ALU.mult, op1=ALU.mult),
                     r=[r_bank[b0], r_cs], w=[r_t1])
                P.op("dve", lambda e: e.scalar_tensor_tensor(out=t2, in0=bb, scalar=scale, in1=sin,
                                                             op0=ALU.mult, op1=ALU.mult),
                     r=[r_bank[b1], r_cs], w=[r_t2])
            P.op("pool", lambda e: e.tensor_tensor(out=dst, in0=t1, in1=t2, op=ALU.add),
                 r=[r_t1, r_t2], w=[r_dst])

        def rope_perm(b0, m, pm, r_pm, cos, sin, r_cs, dst, r_dst, xo, r_xo, t1, r_t1, t2, r_t2, scale=None):
            evac("act", xo[0:m, :], banks[b0][0:m, :], [r_bank[b0]], [r_xo])

            def finish():
                b1 = nbank()
                P.op("pe", lambda e: e.matmul(banks[b1][0:m, :], lhsT=pm[0:m, 0:m], rhs=xo[0:m, :], start=True, stop=True),
                     r=[r_xo, r_pm], w=[r_bank[b1]])
                rope_combine(b0, b1, m, cos, sin, r_cs, dst, r_dst, t1, r_t1, t2, r_t2, scale=scale)
            return finish

        def make_swapped(eng, wt, r_wt, src0, dst0, nheads, hd, half, nchunk):
            n = nheads * hd
            P.op(eng, lambda e: e.tensor_copy(out=wt[:, :, dst0:dst0 + n], in_=wt[:, :, src0:src0 + n]),
                 r=[r_wt], w=[r_wt])
            sv = wt[:, :, src0:src0 + n].rearrange("p c (h d) -> p c h d", h=nheads)
            dv = wt[:, :, dst0:dst0 + n].rearrange("p c (h d) -> p c h d", h=nheads)
            for c in range(nchunk):
                P.op(eng, lambda e, c=c: e.tensor_copy(out=dv[:, c, :, 0:half], in_=sv[:, c, :, half:2 * half]),
                     r=[r_wt], w=[r_wt])
                P.op(eng, lambda e, c=c: e.tensor_copy(out=dv[:, c, :, half:2 * half], in_=sv[:, c, :, 0:half]),
                     r=[r_wt], w=[r_wt])

        w_in_v = w_in.rearrange("(c p) n -> p c n", p=128)

        def load_cols(dst, r_dst, vec, nchunk):
            v = vec.rearrange("(c p o) -> c p o", p=128, o=1)
            for c in range(nchunk):
                P.dma("sp", dst[:, c:c + 1], v[c], w=[r_dst])

        M.push()
        kiT2 = M.alloc(BF16, [S]); r_kiT2 = Reg("kiT2")
        kpeT = M.alloc(BF16, [S], 0, 32); r_kpeT = Reg("kpeT")
        if debug:
            P.op("pool", lambda e: e.memset(kiT2, 0.0), w=[r_kiT2])
            P.op("pool", lambda e: e.memset(kpeT, 0.0), w=[r_kpeT])
        NK = 2112
        Wk = M.alloc(BF16, [8, NK]); r_Wk = Reg("Wk")
        Wuk = M.alloc(BF16, [2, 512]); r_Wuk = Reg("Wuk")
        Wuv = M.alloc(BF16, [2, 512]); r_Wuv = Reg("Wuv")
        gkv = M.alloc(F32, [2]); r_gkv = Reg("gkv")
        C_CKV, C_KR, C_KRS, C_KB, C_KBS, C_VB, C_KI, C_KIS = 0, 256, 288, 320, 832, 1344, 1856, 1984
        for (dst, src, n) in ((C_CKV, 384, 256), (C_KR, 640, 32), (C_KB, 1184, 512), (C_VB, 1696, 512),
                              (C_KI, 2720, 64), (C_KI + 64, 2720, 64)):
            for c0 in range(0, 8, 4):
                P.dma("pool", Wk[:, c0:c0 + 4, dst:dst + n], w_in_v[:, c0:c0 + 4, src:src + n], w=[r_Wk])
        P.dma("pool", Wuk, w_uk.rearrange("(c p) n -> p c n", p=128), w=[r_Wuk])
        P.dma("pool", Wuv, w_uv.rearrange("(c p) n -> p c n", p=128), w=[r_Wuv])
        load_cols(gkv, r_gkv, g_kv, 2)
        xos = [(M.alloc(BF16, [512]), Reg(f"xoA{i}")) for i in range(2)]

        if upto == "W":
            P.dma("sp", kbT_d[0, :, 0:NK * 2], Wk[:, 0:2, :].rearrange("p c n -> p (c n)"), r=[r_Wk], w=[r_dbg])
            P.emit()
            return nc
        zt = M.alloc(BF16, [S], 0, 32); r_zt = Reg("zt")
        P.op("pool", lambda e: e.memset(zt, 0.0), w=[r_zt])
        for h8 in range(8):
            P.dma("sp", knT_d[h8, 96:128, :], zt, r=[r_zt], w=[r_knT_d])
        xbufs = mk_xbufs(4)
        hTs = [(M.alloc(BF16, [8, 512]), Reg(f"hT{i}")) for i in range(2)]
        cs16 = [(M.alloc(F32, [2, 512]), Reg(f"cs16_{i}")) for i in range(2)]
        cs32 = [(M.alloc(F32, [2, 512], 0, 32), Reg(f"cs32_{i}")) for i in range(2)]
        ckvf = M.alloc(F32, [2, 512]); r_ckvf = Reg("ckvf")
        sq = M.alloc(BF16, [2, 512]); r_sq = Reg("sq")
        sd = M.alloc(F32, [512]); r_sd = Reg("sd")
        rstdb = M.alloc(F32, [512]); r_rstdb = Reg("rstdb")
        ckvn = M.alloc(BF16, [2, 512]); r_ckvn = Reg("ckvn")
        t1s = [(M.alloc(F32, [512]), Reg(f"t1_{i}")) for i in range(2)]
        t2s = [(M.alloc(F32, [512]), Reg(f"t2_{i}")) for i in range(2)]
        obufs = [(M.alloc(BF16, [512]), Reg(f"ob{i}")) for i in range(4)]
        vaugs = [(M.alloc(BF16, [4, 8, 128]), Reg(f"vaug{i}")) for i in range(2)]
        for va_, r_va in vaugs:
            P.op("pool", lambda e, va_=va_: e.memset(va_, 1.0), w=[r_va])
        cnt = {"ob": 0, "t": 0}

        def next_ob():
            cnt["ob"] += 1
            return obufs[cnt["ob"] % 4]

        def next_t():
            cnt["t"] += 1
            return t1s[cnt["t"] % 2] + t2s[cnt["t"] % 2]

        def flush_pend(pend, tok0, keep):
            while len(pend) > keep:
                kind, fin, kb_info, _ = pend.pop(0)
                fin()
                if kind == "kr":
                    for h8 in range(8):
                        P.dma("sp", knT_d[h8, 64:96, tok0:tok0 + 512], kpeT[:, tok0:tok0 + 512], r=[r_kpeT], w=[r_knT_d])
                else:
                    hp_, ob_, r_ob_ = kb_info
                    P.dma("sp", kbT_d[hp_, :, tok0:tok0 + 512], ob_, r=[r_ob_], w=[r_kbT_d])

        xb_v = xb.rearrange("(n p) d -> n p d", p=128)
        nblk_a = 16 if upto != "A1" else 1
        if _os.environ.get("KSKIPA"):
            nblk_a = 0
        if _os.environ.get("KNBLKA"):
            nblk_a = int(_os.environ["KNBLKA"])
        MOEONLY = bool(_os.environ.get("KMOEONLY"))
        if MOEONLY:
            nblk_a = 0
        def blockA_pre(blk, part="both"):
            hT, r_hT = hTs[blk % 2]
            c16, r_c16 = cs16[blk % 2]
            c32, r_c32 = cs32[blk % 2]
            tok0 = blk * 512
            if part in ("both", "stats"):
                P.dma("sp", c16, cs16k[:, :, tok0:tok0 + 512].rearrange("a p n -> p a n"), w=[r_c16])
                P.dma("sp", c32, cs32k[:, :, tok0:tok0 + 512].rearrange("a p n -> p a n"), w=[r_c32])
            norm_transpose(lambda t: xb_v[blk * 4 + t], gmix_b, r_gmix, xbufs, hT, r_hT, blk, part=part)

        def blockA_proj(blk):
            hT, r_hT = hTs[blk % 2]
            c16, r_c16 = cs16[blk % 2]
            c32, r_c32 = cs32[blk % 2]
            tok0 = blk * 512
            for cc in range(2):
                b = proj_T(Wk, r_Wk, C_CKV + cc * 128, 128, hT, r_hT, 8)
                P.op("act", lambda e, b=b, cc=cc: e.activation(out=sq[:, cc, :], in_=banks[b], func=AF.Square),
                     r=[r_bank[b]], w=[r_sq])
                P.op("dve", lambda e, b=b, cc=cc: e.tensor_copy(out=ckvf[:, cc, :], in_=banks[b]),
                     r=[r_bank[b]], w=[r_ckvf])
            b = nbank()
            for cc in range(2):
                P.op("pe", lambda e, b=b, cc=cc: e.matmul(banks[b], lhsT=ones_b, rhs=sq[:, cc, :],
                                                          start=(cc == 0), stop=(cc == 1)),
                     r=[r_ones, r_sq], w=[r_bank[b]])
            P.op("act", lambda e, b=b: e.activation(out=sd, in_=banks[b], func=AF.Sqrt, scale=1.0 / 256, bias=EPS),
                 r=[r_bank[b]], w=[r_sd])
            P.op("dve", lambda e: e.reciprocal(out=rstdb, in_=sd), r=[r_sd], w=[r_rstdb])
            for cc in range(2):
                P.op("dve", lambda e, cc=cc: e.scalar_tensor_tensor(
                    out=ckvn[:, cc, :], in0=ckvf[:, cc, :], scalar=gkv[:, cc:cc + 1], in1=rstdb,
                    op0=ALU.mult, op1=ALU.mult), r=[r_ckvf, r_gkv, r_rstdb], w=[r_ckvn])
            b0 = proj_T(Wk, r_Wk, C_KR, 32, hT, r_hT, 8)
            t1, r_t1, t2, r_t2 = next_t()
            xo, r_xo = xos[0]
            fin_kr = rope_perm(b0, 32, pm32, r_pm32, c32[:, 0, :], c32[:, 1, :], r_c32, kpeT[:, tok0:tok0 + 512], r_kpeT,
                               xo, r_xo, t1[0:32, :], r_t1, t2[0:32, :], r_t2)
            pend = [("kr", fin_kr, None, None)]
            for hp in range(4):
                b0 = proj_T(Wk, r_Wk, C_KB + hp * 128, 128, hT, r_hT, 8)
                ob, r_ob = next_ob()
                t1, r_t1, t2, r_t2 = next_t()
                xo, r_xo = xos[(hp + 1) % 2]
                fin = rope_perm(b0, 128, pm16, r_pm16, c16[:, 0, :], c16[:, 1, :], r_c16, ob, r_ob, xo, r_xo, t1, r_t1, t2, r_t2)
                pend.append(("kb", fin, (hp, ob, r_ob), None))
                flush_pend(pend, tok0, 1)
            if blk + 1 < nblk_a:
                blockA_pre(blk + 1, part="stats")
            vb_, r_vb = vaugs[1]
            for t in range(4):
                b = nbank()
                for c in range(8):
                    P.op("pe", lambda e, b=b, c=c, t=t, hT=hT: e.matmul(
                        banks[b], lhsT=hT[:, c, t * 128:(t + 1) * 128], rhs=Wk[:, c, C_VB:C_VB + 512],
                        start=(c == 0), stop=(c == 7)), r=[r_hT, r_Wk], w=[r_bank[b]])
                src4 = banks[b].rearrange("p (hp hh e) -> p hp hh e", hh=2, e=64)
                dst4 = vb_[:, t, :, :].rearrange("p (hp hh) e -> p hp hh e", hh=2)
                evac("act", dst4[:, :, 0, 0:64], src4[:, :, 0, :], [r_bank[b]], [r_vb])
                evac("dve", dst4[:, :, 1, 64:128], src4[:, :, 1, :], [r_bank[b]], [r_vb])
            P.dma("sp", vb_d[tok0:tok0 + 512, :].rearrange("(t p) n -> p t n", p=128),
                  vb_.rearrange("p t h e -> p t (h e)"), r=[r_vb], w=[r_vb_d])
            b0 = proj_T(Wk, r_Wk, C_KI, 128, hT, r_hT, 8)
            t1, r_t1, t2, r_t2 = next_t()
            flush_pend(pend, tok0, 0)
            xo, r_xo = xos[0]
            fin = rope_perm(b0, 128, pm16, r_pm16, c16[:, 0, :], c16[:, 1, :], r_c16, kiT2[:, tok0:tok0 + 512], r_kiT2,
                            xo, r_xo, t1, r_t1, t2, r_t2)
            fin()
            for hp in range(4):
                b = proj_T(Wuk, r_Wuk, hp * 128, 128, ckvn, r_ckvn, 2)
                ob, r_ob = next_ob()
                evac("act", ob, banks[b], [r_bank[b]], [r_ob])
                P.dma("sp", knT_d[2 * hp, 0:64, tok0:tok0 + 512], ob[0:64, :], r=[r_ob], w=[r_knT_d])
                P.dma("sp", knT_d[2 * hp + 1, 0:64, tok0:tok0 + 512], ob[64:128, :], r=[r_ob], w=[r_knT_d])
            va_, r_va = vaugs[0]
            for t in range(4):
                b = nbank()
                for cc in range(2):
                    P.op("pe", lambda e, b=b, cc=cc, t=t: e.matmul(
                        banks[b], lhsT=ckvn[:, cc, t * 128:(t + 1) * 128], rhs=Wuv[:, cc, :],
                        start=(cc == 0), stop=(cc == 1)), r=[r_ckvn, r_Wuv], w=[r_bank[b]])
                src4 = banks[b].rearrange("p (hp hh e) -> p hp hh e", hh=2, e=64)
                dst4 = va_[:, t, :, :].rearrange("p (hp hh) e -> p hp hh e", hh=2)
                evac("act", dst4[:, :, 0, 0:64], src4[:, :, 0, :], [r_bank[b]], [r_va])
                evac("dve", dst4[:, :, 1, 64:128], src4[:, :, 1, :], [r_bank[b]], [r_va])
            P.dma("sp", va_d[tok0:tok0 + 512, :].rearrange("(t p) n -> p t n", p=128),
                  va_.rearrange("p t h e -> p t (h e)"), r=[r_va], w=[r_va_d])

        if nblk_a > 0:
            blockA_pre(0)
        for blk in range(nblk_a):
            blockA_proj(blk)
            if blk + 1 < nblk_a:
                blockA_pre(blk + 1, part="T")
        P.dma("sp", ki_d, kiT2, r=[r_kiT2], w=[r_ki_d])
        P.dma("sp", kpe_d, kpeT, r=[r_kpeT], w=[r_kpe_d])
        M.pop()
        P.barrier()
        if upto in ("A", "A1"):
            P.emit()
            return nc


        M.push()
        NQW = 4488
        Wq = M.alloc(BF16, [8, NQW]); r_Wq = Reg("Wq")
        Wun = M.alloc(BF16, [3, 512]); r_Wun = Reg("Wun")
        Wur = M.alloc(BF16, [3, 512]); r_Wur = Reg("Wur")
        gq = M.alloc(F32, [3]); r_gq = Reg("gq")
        Q_CQ, Q_QB, Q_QBS, Q_QI, Q_QIS, Q_WI, Q_GA, Q_GB = 0, 384, 896, 1408, 1920, 2432, 2440, 3464
        for (dst, src, n) in ((Q_CQ, 0, 384), (Q_QB, 672, 512), (Q_QI, 2208, 512), (Q_WI, 2784, 8),
                              (Q_GA, 2792, 1024), (Q_GB, 3816, 1024)):
            for c0 in range(0, 8, 4):
                P.dma("pool", Wq[:, c0:c0 + 4, dst:dst + n], w_in_v[:, c0:c0 + 4, src:src + n], w=[r_Wq])
        w_uq_v = w_uq.rearrange("(c p) (h e) -> p c h e", p=128, h=8)
        for c in range(3):
            P.dma("pool", Wun[:, c, :].rearrange("p (h e) -> p h e", h=8), w_uq_v[:, c, :, 0:64], w=[r_Wun])
            P.dma("pool", Wur[:, c, 0:256].rearrange("p (h e) -> p h e", h=8), w_uq_v[:, c, :, 64:96], w=[r_Wur])
        load_cols(gq, r_gq, g_q, 3)
        xoq = [(M.alloc(BF16, [512]), Reg(f"xoQ{i}")) for i in range(2)]
        c32q4 = M.alloc(F32, [2, 512]); r_c32q4 = Reg("c32q4")
        xbufs = mk_xbufs(4)
        hTq = M.alloc(BF16, [8, 512]); r_hTq = Reg("hTq")
        c16q = M.alloc(F32, [2, 512]); r_c16q = Reg("c16q")
        c32q = M.alloc(F32, [2, 512], 0, 32); r_c32q = Reg("c32q")
        cqf = M.alloc(F32, [3, 512]); r_cqf = Reg("cqf")
        sq3 = M.alloc(BF16, [3, 512]); r_sq3 = Reg("sq3")
        sdq = M.alloc(F32, [512]); r_sdq = Reg("sdq")
        rstdq = M.alloc(F32, [512]); r_rstdq = Reg("rstdq")
        cqn = M.alloc(BF16, [3, 512]); r_cqn = Reg("cqn")
        t1s = [(M.alloc(F32, [512]), Reg(f"qt1_{i}")) for i in range(2)]
        t2s = [(M.alloc(F32, [512]), Reg(f"qt2_{i}")) for i in range(2)]
        qb_t = M.alloc(BF16, [4, 512]); r_qb_t = Reg("qb_t")
        qi_t = M.alloc(BF16, [4, 512]); r_qi_t = Reg("qi_t")
        qn_t = M.alloc(BF16, [4, 512]); r_qn_t = Reg("qn_t")
        qpe_t = M.alloc(BF16, [2, 512]); r_qpe_t = Reg("qpe_t")
        wt_t = M.alloc(F32, [4, 8]); r_wt_t = Reg("wt_t")
        g_t = [(M.alloc(BF16, [8, 512]), Reg(f"g_t{i}")) for i in range(2)]
        xq_v = xq.rearrange("(n p) d -> n p d", p=128)
        SC_A = 96 ** -0.5
        for s in range(0 if MOEONLY else (4 if (upto != "Q1" and not _os.environ.get("KQ1")) else 1)):
            q0 = s * 512
            P.dma("sp", c16q, cs16q[:, :, q0:q0 + 512].rearrange("a p n -> p a n"), w=[r_c16q])
            P.dma("sp", c32q4, cs32q4[:, :, q0:q0 + 512].rearrange("a p n -> p a n"), w=[r_c32q4])
            norm_transpose(lambda t, s=s: xq_v[s * 4 + t], gmix_b, r_gmix, xbufs, hTq, r_hTq, s)
            for cc in range(3):
                b = proj_T(Wq, r_Wq, Q_CQ + cc * 128, 128, hTq, r_hTq, 8)
                P.op("act", lambda e, b=b, cc=cc: e.activation(out=sq3[:, cc, :], in_=banks[b], func=AF.Square),
                     r=[r_bank[b]], w=[r_sq3])
                P.op("dve", lambda e, b=b, cc=cc: e.tensor_copy(out=cqf[:, cc, :], in_=banks[b]),
                     r=[r_bank[b]], w=[r_cqf])
            b = nbank()
            for cc in range(3):
                P.op("pe", lambda e, b=b, cc=cc: e.matmul(banks[b], lhsT=ones_b, rhs=sq3[:, cc, :],
                                                          start=(cc == 0), stop=(cc == 2)),
                     r=[r_ones, r_sq3], w=[r_bank[b]])
            P.op("act", lambda e, b=b: e.activation(out=sdq, in_=banks[b], func=AF.Sqrt, scale=1.0 / 384, bias=EPS),
                 r=[r_bank[b]], w=[r_sdq])
            P.op("dve", lambda e: e.reciprocal(out=rstdq, in_=sdq), r=[r_sdq], w=[r_rstdq])
            for cc in range(3):
                P.op("dve", lambda e, cc=cc: e.scalar_tensor_tensor(
                    out=cqn[:, cc, :], in0=cqf[:, cc, :], scalar=gq[:, cc:cc + 1], in1=rstdq,
                    op0=ALU.mult, op1=ALU.mult), r=[r_cqf, r_gq, r_rstdq], w=[r_cqn])
            qpend = []
            items = [("qb", hp) for hp in range(4)] + [("qi", hp) for hp in range(4)] + [("pe", g4) for g4 in range(2)]
            for ii, (kind, j) in enumerate(items):
                t1, r_t1 = t1s[ii % 2]; t2, r_t2 = t2s[ii % 2]
                xo, r_xo = xoq[ii % 2]
                if kind == "pe":
                    b0 = proj_T(Wur, r_Wur, j * 128, 128, cqn, r_cqn, 3)
                    fin = rope_perm(b0, 128, pm32, r_pm32, c32q4[:, 0, :], c32q4[:, 1, :], r_c32q4, qpe_t[:, j, :], r_qpe_t,
                                    xo, r_xo, t1, r_t1, t2, r_t2, scale=SC_A)
                elif kind == "qb":
                    b0 = proj_T(Wq, r_Wq, Q_QB + j * 128, 128, hTq, r_hTq, 8)
                    fin = rope_perm(b0, 128, pm16, r_pm16, c16q[:, 0, :], c16q[:, 1, :], r_c16q, qb_t[:, j, :], r_qb_t,
                                    xo, r_xo, t1, r_t1, t2, r_t2, scale=0.125)
                else:
                    b0 = proj_T(Wq, r_Wq, Q_QI + j * 128, 128, hTq, r_hTq, 8)
                    fin = rope_perm(b0, 128, pm16, r_pm16, c16q[:, 0, :], c16q[:, 1, :], r_c16q, qi_t[:, j, :], r_qi_t,
                                    xo, r_xo, t1, r_t1, t2, r_t2)
                qpend.append(fin)
                if len(qpend) > 1:
                    qpend.pop(0)()
            while qpend:
                qpend.pop(0)()
            for t in range(4):
                b = nbank()
                for c in range(8):
                    P.op("pe", lambda e, b=b, c=c, t=t: e.matmul(
                        banks[b][:, 0:8], lhsT=hTq[:, c, t * 128:(t + 1) * 128], rhs=Wq[:, c, Q_WI:Q_WI + 8],
                        start=(c == 0), stop=(c == 7)), r=[r_hTq, r_Wq], w=[r_bank[b]])
                evac("act", wt_t[:, t, :], banks[b][:, 0:8], [r_bank[b]], [r_wt_t], scale=(8 ** -0.5) * 0.125)
            for which in range(2):
                gt, r_gt = g_t[which]
                for dch in range(8):
                    b = proj_T(Wq, r_Wq, (Q_GA, Q_GB)[which] + dch * 128, 128, hTq, r_hTq, 8)
                    evac("act", gt[:, dch, :], banks[b], [r_bank[b]], [r_gt], func=AF.Sigmoid)
            for hp in range(4):
                b = proj_T(Wun, r_Wun, hp * 128, 128, cqn, r_cqn, 3)
                evac("act", qn_t[:, hp, :], banks[b], [r_bank[b]], [r_qn_t], scale=SC_A)
            P.dma("pool", qb_d[s], qb_t.rearrange("p a n -> p (a n)"), r=[r_qb_t], w=[r_q_d[s]])
            P.dma("pool", qi_d[s], qi_t.rearrange("p a n -> p (a n)"), r=[r_qi_t], w=[r_q_d[s]])
            P.dma("pool", qn_d[s], qn_t.rearrange("p a n -> p (a n)"), r=[r_qn_t], w=[r_q_d[s]])
            for h8 in range(8):
                g4, hl = divmod(h8, 4)
                P.dma("pool", qpe_d[s, :, h8 * 512:(h8 + 1) * 512], qpe_t[hl * 32:(hl + 1) * 32, g4, :],
                      r=[r_qpe_t], w=[r_q_d[s]])
            P.dma("pool", wt_d[s], wt_t.rearrange("p a n -> p (a n)"), r=[r_wt_t], w=[r_q_d[s]])
            P.dma("pool", ga_d[s], g_t[0][0].rearrange("p a n -> p (a n)"), r=[g_t[0][1]], w=[r_q_d[s]])
            P.dma("pool", gb_d[s], g_t[1][0].rearrange("p a n -> p (a n)"), r=[g_t[1][1]], w=[r_q_d[s]])
        M.pop()
        P.barrier()
        if upto in ("Q", "Q1"):
            P.emit()
            return nc

        M.push()
        kiT2 = M.alloc(BF16, [S]); r_kiT2 = Reg("kiT2b")
        P.dma("sp", kiT2, ki_d, r=[r_ki_d], w=[r_kiT2])
        qi_s = M.alloc(BF16, [8, 512]); r_qi_s = Reg("qi_s")
        P.op("pool", lambda e: e.memset(qi_s, 0.0), w=[r_qi_s])
        wt_s = M.alloc(F32, [4, 8]); r_wt_s = Reg("wt_s")
        o_a = M.alloc(BF16, [4, 512]); r_o_a = Reg("o_a")
        o_b = M.alloc(BF16, [4, 512]); r_o_b = Reg("o_b")
        pw2 = M.alloc(F32, [NITER]); r_pw2 = Reg("pw2")
        bis = M.alloc(F32, [8 + NITER]); r_bis = Reg("bis")
        bisB = M.alloc(F32, [4]); r_bisB = Reg("bisB")
        bisA = M.alloc(F32, [4]); r_bisA = Reg("bisA")
        selm = M.alloc(F32, [128]); r_selm = Reg("selm")
        P.op("pool", lambda e: e.memset(selm, 0.0), w=[r_selm])
        P.op("pool", lambda e: e.memset(selm[0:1, :], 1.0), r=[r_selm], w=[r_selm])
        P.op("pool", lambda e: e.memset(selm[64:65, :], 1.0), r=[r_selm], w=[r_selm])
        att_ev = [(M.alloc(F32, [512]), Reg(f"att_ev{i}")) for i in range(2)]
        att_rs = [(M.alloc(F32, [512]), Reg(f"att_rs{i}")) for i in range(2)]
        for k in range(NITER):
            P.op("pool", lambda e, k=k: e.memset(pw2[:, k:k + 1], 2.0 ** (-k)), w=[r_pw2])
        rr["pool"] = [0, 1, 2, 3, 4, 5]
        kvstate = {"n": 0}
        nslots = 4 if upto not in ("I1", "T1", "F1") else 1
        if MOEONLY:
            nslots = 0
        if _os.environ.get("KNSLOT"):
            nslots = int(_os.environ["KNSLOT"])

        def attention(s, hp, kT_d, r_kT_d, v_d, r_v_d, q_s, r_q_s, o_t, r_o_t, mla, maskT, r_maskT, kbufs, vbufs, pts, cmT, r_cmT, kpeT=None, r_kpeT=None, qpe_s=None, r_qpe_s=None):
            tiles = [(c, t, hh) for c in range(s + 1) for t in range(16) for hh in range(2)]
            loaded = {}
            state = {"n": 0}

            def load(c):
                if c in loaded:
                    return loaded[c]
                kvi = kvstate["n"] % len(kbufs); kvstate["n"] += 1
                kb_, r_kb_ = kbufs[kvi]; vb_, r_vb_ = vbufs[kvi]
                if mla:
                    P.dma("sp", kb_, kT_d[2 * hp:2 * hp + 2, :, c * 2048:(c + 1) * 2048].rearrange("h p n -> p h n"),
                          r=[r_kT_d], w=[r_kb_])
                else:
                    P.dma("sp", kb_[:, 0, :], kT_d[hp, :, c * 2048:(c + 1) * 2048], r=[r_kT_d], w=[r_kb_])
                P.dma("sp", vb_, v_d[c * 2048:(c + 1) * 2048, hp * 256:(hp + 1) * 256].rearrange(
                    "(t p) e -> p t e", p=128), r=[r_v_d], w=[r_vb_])
                loaded[c] = (kb_, r_kb_, vb_, r_vb_)
                return loaded[c]

            def stage1(i):
                c, t, hh = tiles[i]
                kt = c * 16 + t
                kb_, r_kb_, vb_, r_vb_ = load(c)
                b = nbank()
                pt, r_pt = pts[i % len(pts)]
                hsel = hh if mla else 0
                P.op("pe", lambda e: e.matmul(
                    banks[b], lhsT=kb_[:, hsel, t * 128:(t + 1) * 128],
                    rhs=q_s[:, hp * 2 + hh, :], start=True, stop=True),
                    r=[r_kb_, r_q_s], w=[r_bank[b]])
                P.op("act", lambda e: e.activation(out=pt, in_=banks[b], func=AF.Exp), r=[r_bank[b]], w=[r_pt])
                eng = "dve" if i % 2 == 0 else "pool"
                if not mla:
                    P.op(eng, lambda e: e.tensor_tensor(out=pt, in0=pt, in1=maskT[:, kt, :], op=ALU.mult),
                         r=[r_pt, r_maskT], w=[r_pt])
                elif c == s:
                    P.op(eng, lambda e: e.tensor_tensor(out=pt, in0=pt, in1=cmT[:, t, :], op=ALU.mult),
                         r=[r_pt, r_cmT], w=[r_pt])

            def stage2(i):
                c, t, hh = tiles[i]
                kb_, r_kb_, vb_, r_vb_ = loaded[c]
                pt, r_pt = pts[i % len(pts)]
                first = (c == 0 and t == 0)
                last = (c == s and t == 15)
                P.op("pe", lambda e: e.matmul(
                    banks[6 + hh], lhsT=vb_[:, t, hh * 128:(hh + 1) * 128], rhs=pt, start=first, stop=last),
                    r=[r_pt, r_vb_], w=[r_bank[6 + hh]])

            DEPTH = 6
            n = len(tiles)
            for i in range(0, n + DEPTH, 2):
                for j in (i, i + 1):
                    if j < n:
                        stage1(j)
                for j in (i - DEPTH, i - DEPTH + 1):
                    if 0 <= j < n:
                        stage2(j)
            for hh in range(2):
                srow = slice(64, 128) if hh == 0 else slice(0, 64)
                orow = slice(0, 64) if hh == 0 else slice(64, 128)
                evs, r_evs = att_ev[hh]
                rsb, r_rsb = att_rs[hh]
                P.op("act", lambda e, hh=hh, srow=srow, evs=evs: e.activation(
                    out=evs[srow, :], in_=banks[6 + hh][srow, :], func=AF.Copy), r=[r_bank[6 + hh]], w=[r_evs])
                b = nbank()
                P.op("pe", lambda e, b=b, srow=srow, evs=evs: e.matmul(
                    banks[b], lhsT=selm[srow, :], rhs=evs[srow, :], start=True, stop=True),
                    r=[r_evs, r_selm], w=[r_bank[b]])
                P.op("dve", lambda e, b=b, orow=orow, rsb=rsb: e.reciprocal(out=rsb[orow, :], in_=banks[b][orow, :]),
                     r=[r_bank[b]], w=[r_rsb])
                P.op("dve", lambda e, hh=hh, orow=orow, rsb=rsb: e.tensor_tensor(
                    out=o_t[orow, hp, :], in0=banks[6 + hh][orow, :], in1=rsb[orow, :], op=ALU.mult),
                    r=[r_bank[6 + hh], r_rsb], w=[r_o_t])

        def do_slot(s):
            ext = 2048 * (s + 1)
            nkt = 16 * (s + 1)
            for hh_ in range(2):
                rws = slice(hh_ * 64, (hh_ + 1) * 64)
                P.dma("sp", qi_s[rws].rearrange("p (hp hh) n -> p hp hh n", hh=2)[:, :, hh_, :],
                      qi_d[s, rws, :].rearrange("p (hp n) -> p hp n", hp=4), r=[r_q_d[s]], w=[r_qi_s])
            P.dma("sp", wt_s.rearrange("p a n -> p (a n)"), wt_d[s], r=[r_q_d[s]], w=[r_wt_s])
            M.push()
            maskT = M.alloc(BF16, [64, 512]); r_maskT = Reg(f"maskT{s}")
            M.push()
            score = M.alloc(F32, [S]); r_score = Reg(f"score{s}")
            moff = M.off
            mrow = M.alloc(BF16, [S]); r_mrow = Reg(f"mrow{s}"); r_mrowB = Reg(f"mrowB{s}")
            cmrow = M.view(moff, F32, [2048])
            rls = [(M.alloc(F32, [512]), Reg(f"rl{s}_{i}")) for i in range(4)]
            nkg = 4 * (s + 1)
            r_sc = [Reg(f"score{s}_{i}") for i in range(nkg)]
            for qt in range(4):
                n = 0
                for hp_, kgi, hh in [(a_, b_, c_) for a_ in range(4) for b_ in range(nkg) for c_ in range(2)]:
                    h = hp_ * 2 + hh
                    if True:
                        cols = slice(kgi * 512, kgi * 512 + 512)
                        b = nbank()
                        rl, r_rl = rls[n % 4]; n += 1
                        P.op("pe", lambda e, b=b, hp_=hp_, hh=hh, cols=cols, qt=qt: e.matmul(
                            banks[b], lhsT=qi_s[:, hp_ * 2 + hh, qt * 128:(qt + 1) * 128],
                            rhs=kiT2[:, cols], start=True, stop=True),
                            r=[r_qi_s, r_kiT2], w=[r_bank[b]])
                        P.op("act", lambda e, b=b, rl=rl: e.activation(out=rl, in_=banks[b], func=AF.Relu),
                             r=[r_bank[b]], w=[r_rl])
                        if h == 0:
                            P.op("dve", lambda e, rl=rl, cols=cols, qt=qt: e.tensor_scalar(
                                out=score[:, cols], in0=rl, scalar1=wt_s[:, qt, 0:1], scalar2=None,
                                op0=ALU.mult), r=[r_rl, r_wt_s], w=[r_sc[kgi]])
                        else:
                            P.op("dve", lambda e, rl=rl, cols=cols, qt=qt, h=h: e.scalar_tensor_tensor(
                                out=score[:, cols], in0=rl, scalar=wt_s[:, qt, h:h + 1], in1=score[:, cols],
                                op0=ALU.mult, op1=ALU.add), r=[r_rl, r_wt_s, r_sc[kgi]], w=[r_sc[kgi]])
                r_score_l = r_sc
                P.op("dve", lambda e: e.tensor_reduce(out=bis[:, 0:1], in_=score[:, 0:ext], axis=AX.X, op=ALU.max,
                                                      apply_absolute_value=True), r=r_sc, w=[r_bis])
                P.op("dve", lambda e: e.tensor_scalar(out=bis[:, 5:6], in0=bis[:, 0:1], scalar1=1.001, scalar2=1e-6,
                                                      op0=ALU.mult, op1=ALU.add), r=[r_bis], w=[r_bis])
                P.op("dve", lambda e: e.tensor_scalar(out=bis[:, 8:8 + NITER], in0=pw2, scalar1=bis[:, 5:6], scalar2=None,
                                                      op0=ALU.mult), r=[r_bis, r_pw2], w=[r_bis])
                P.op("dve", lambda e: e.memset(bis[:, 2:3], 0.0), r=[r_bis], w=[r_bis])
                P.dma("sp", cmrow, cmask[qt], w=[r_mrow, r_mrowB])
                P.op("dve", lambda e: e.tensor_tensor(out=score[:, s * 2048:(s + 1) * 2048],
                                                      in0=score[:, s * 2048:(s + 1) * 2048], in1=cmrow, op=ALU.add),
                     r=r_sc[4 * s:] + [r_mrow], w=r_sc[4 * s:])
                cA = (ext * 27 // 64) // 64 * 64
                nB = ext - cA
                for k in range(NITER):
                    P.op("dve", lambda e: e.tensor_scalar(out=mrow[:, 0:cA], in0=score[:, 0:cA], scalar1=bis[:, 2:3],
                                                          scalar2=0.0, op0=ALU.is_ge, op1=ALU.add, accum_out=bisA[:, 0:1]),
                         r=r_sc + [r_bis], w=[r_mrow, r_bisA])
                    P.op("act", lambda e: e.activation(out=mrow[:, cA:ext], in_=score[:, cA:ext], func=AF.Sign,
                                                      scale=-1.0, bias=bis[:, 2:3], accum_out=bisB[:, 0:1]),
                         r=r_sc + [r_bis], w=[r_mrowB, r_bisB])
                    P.op("dve", lambda e: e.scalar_tensor_tensor(
                        out=bis[:, 7:8], in0=bisB[:, 0:1], scalar=-0.5, in1=bisA[:, 0:1], op0=ALU.mult, op1=ALU.add),
                        r=[r_bisA, r_bisB], w=[r_bis])
                    P.op("dve", lambda e: e.tensor_scalar(out=bis[:, 4:5], in0=bis[:, 7:8], scalar1=TOPK - 0.5 - nB / 2.0,
                                                          scalar2=-0.5, op0=ALU.is_ge, op1=ALU.add),
                         r=[r_bis], w=[r_bis])
                    P.op("dve", lambda e, k=k: e.scalar_tensor_tensor(
                        out=bis[:, 2:3], in0=bis[:, 4:5], scalar=bis[:, 8 + k:9 + k], in1=bis[:, 2:3],
                        op0=ALU.mult, op1=ALU.add), r=[r_bis], w=[r_bis])
                P.op("dve", lambda e: e.scalar_tensor_tensor(
                    out=bis[:, 1:2], in0=bis[:, 8 + NITER - 1:8 + NITER], scalar=-0.5, in1=bis[:, 2:3],
                    op0=ALU.mult, op1=ALU.add), r=[r_bis], w=[r_bis])
                P.op("dve", lambda e: e.tensor_scalar(out=mrow[:, 0:ext], in0=score[:, 0:ext], scalar1=bis[:, 1:2],
                                                      scalar2=None, op0=ALU.is_ge), r=r_sc + [r_bis], w=[r_mrow, r_mrowB])
                if debug:
                    P.dma("sp", thr_d[s * 4 + qt], bis[:, 0:2], r=[r_bis], w=[r_dbg])
                for g in range(nkt // 8):
                    b = nbank()
                    tp = banks[b].bitcast(BF16)
                    for j in range(8):
                        kt = g * 8 + j
                        P.op("pe", lambda e, tp=tp, j=j, kt=kt: e.transpose(
                            out=tp[:, j * 128:(j + 1) * 128], in_=mrow[:, kt * 128:(kt + 1) * 128], identity=ident),
                            r=[r_mrow, r_mrowB, r_ident], w=[r_bank[b]])
                    evac("act" if g % 2 == 0 else "dve", maskT[:, g * 8:(g + 1) * 8, qt * 128:(qt + 1) * 128],
                         tp.rearrange("p (j k) -> p j k", j=8), [r_bank[b]], [r_maskT])
            M.pop()
            P.barrier()
            if upto == "I1":
                P.dma("sp", kbT_d[0, :, 0:8192], maskT[:, 0:16, :].rearrange("p a n -> p (a n)"), r=[r_maskT], w=[r_dbg])
                P.emit()
                return True
            M.push()
            kbufs = [(M.alloc(BF16, [2, 2048]), Reg(f"kbuf{s}_{i}")) for i in range(2)]
            vbufs = [(M.alloc(BF16, [16, 256]), Reg(f"vbuf{s}_{i}")) for i in range(2)]
            pts = [(M.alloc(BF16, [512]), Reg(f"pt{s}_{i}")) for i in range(8)]
            cmT = M.alloc(BF16, [16, 512]); r_cmT = Reg(f"cmT{s}")
            P.dma("pool", cmT, cmaskT, w=[r_cmT])
            qb_s = M.alloc(BF16, [8, 512]); r_qb_s = Reg(f"qb_s{s}")
            qa_s = M.alloc(BF16, [8, 512]); r_qa_s = Reg(f"qa_s{s}")
            P.op("pool", lambda e: e.memset(qb_s, 0.0), w=[r_qb_s])
            P.op("pool", lambda e: e.memset(qa_s[96:128], 0.0), w=[r_qa_s])
            for hh_ in range(2):
                rws = slice(hh_ * 64, (hh_ + 1) * 64)
                P.dma("sp", qb_s[rws].rearrange("p (hp hh) n -> p hp hh n", hh=2)[:, :, hh_, :],
                      qb_d[s, rws, :].rearrange("p (hp n) -> p hp n", hp=4), r=[r_q_d[s]], w=[r_qb_s])
                P.dma("sp", qa_s[0:64].rearrange("p (hp hh) n -> p hp hh n", hh=2)[:, :, hh_, :],
                      qn_d[s, rws, :].rearrange("p (hp n) -> p hp n", hp=4), r=[r_q_d[s]], w=[r_qa_s])
            P.dma("sp", qa_s[64:96].rearrange("p a n -> p (a n)"), qpe_d[s], r=[r_q_d[s]], w=[r_qa_s])
            for hp in range(0 if _os.environ.get("KSKIPDSA") else 4):
                attention(s, hp, kbT_d, r_kbT_d, vb_d, r_vb_d, qb_s, r_qb_s, o_b, r_o_b, False, maskT, r_maskT,
                          kbufs, vbufs, pts, cmT, r_cmT)
            for hp in range(0 if _os.environ.get("KSKIPMLA") else 4):
                attention(s, hp, knT_d, r_knT_d, va_d, r_va_d, qa_s, r_qa_s, o_a, r_o_a, True, maskT, r_maskT,
                          kbufs, vbufs, pts, cmT, r_cmT)
            M.pop()
            M.pop()
            P.barrier()
            if debug:
                P.dma("sp", ob_d[s], o_b.rearrange("p a n -> p (a n)"), r=[r_o_b], w=[r_dbg])
                P.dma("sp", oa_d[s], o_a.rearrange("p a n -> p (a n)"), r=[r_o_a], w=[r_dbg])
            if upto == "T1":
                P.emit()
                return True

            rr["pool"] = list(range(8))
            M.push()
            Woa = M.alloc(BF16, [4, 1024]); r_Woa = Reg(f"Woa{s}")
            Wob = M.alloc(BF16, [4, 1024]); r_Wob = Reg(f"Wob{s}")
            Wout = M.alloc(BF16, [8, 1024]); r_Wout = Reg(f"Wout{s}")
            if s == 0:
                P.dma("pool", Woa, w_o_a.rearrange("(c p) n -> p c n", p=128), w=[r_Woa])
                P.dma("pool", Wob, w_o_b.rearrange("(c p) n -> p c n", p=128), w=[r_Wob])
                for c0 in range(0, 8, 4):
                    P.dma("pool", Wout[:, c0:c0 + 4, :], w_out.rearrange("(c p) n -> p c n", p=128)[:, c0:c0 + 4, :], w=[r_Wout])
                P.dma("sp", wf_d[:, 0:4096], Woa.rearrange("p c n -> p (c n)"), r=[r_Woa], w=[r_wf_d])
                P.dma("sp", wf_d[:, 4096:8192], Wob.rearrange("p c n -> p (c n)"), r=[r_Wob], w=[r_wf_d])
                P.dma("sp", wf_d[:, 8192:16384], Wout.rearrange("p c n -> p (c n)"), r=[r_Wout], w=[r_wf_d])
            else:
                P.dma("sp", Woa.rearrange("p c n -> p (c n)"), wf_d[:, 0:4096], r=[r_wf_d], w=[r_Woa])
                P.dma("sp", Wob.rearrange("p c n -> p (c n)"), wf_d[:, 4096:8192], r=[r_wf_d], w=[r_Wob])
                P.dma("sp", Wout.rearrange("p c n -> p (c n)"), wf_d[:, 8192:16384], r=[r_wf_d], w=[r_Wout])
            ga_s = M.alloc(BF16, [8, 512]); r_ga_s = Reg(f"ga_s{s}")
            gb_s = M.alloc(BF16, [8, 512]); r_gb_s = Reg(f"gb_s{s}")
            P.dma("sp", ga_s.rearrange("p a n -> p (a n)"), ga_d[s], r=[r_q_d[s]], w=[r_ga_s])
            P.dma("sp", gb_s.rearrange("p a n -> p (a n)"), gb_d[s], r=[r_q_d[s]], w=[r_gb_s])
            yT = M.alloc(BF16, [8, 512]); r_yT = Reg(f"yT{s}")
            ft1 = [(M.alloc(F32, [512]), Reg(f"ft1_{s}_{i}")) for i in range(2)]
            ft2 = [(M.alloc(F32, [512]), Reg(f"ft2_{s}_{i}")) for i in range(2)]
            fxt = [(M.alloc(F32, [D]), Reg(f"fxt{s}_{i}")) for i in range(2)]
            fx1 = [(M.alloc(F32, [D]), Reg(f"fx1{s}_{i}")) for i in range(2)]
            for dch in range(8):
                ba = nbank()
                for hp in range(4):
                    P.op("pe", lambda e, ba=ba, hp=hp, dch=dch: e.matmul(
                        banks[ba], lhsT=Woa[:, hp, dch * 128:(dch + 1) * 128], rhs=o_a[:, hp, :],
                        start=(hp == 0), stop=(hp == 3)), r=[r_Woa, r_o_a], w=[r_bank[ba]])
                bb = nbank()
                for hp in range(4):
                    P.op("pe", lambda e, bb=bb, hp=hp, dch=dch: e.matmul(
                        banks[bb], lhsT=Wob[:, hp, dch * 128:(dch + 1) * 128], rhs=o_b[:, hp, :],
                        start=(hp == 0), stop=(hp == 3)), r=[r_Wob, r_o_b], w=[r_bank[bb]])
                t1, r_t1 = ft1[dch % 2]; t2, r_t2 = ft2[dch % 2]
                P.op("dve", lambda e, ba=ba, dch=dch, t1=t1: e.tensor_tensor(out=t1, in0=banks[ba], in1=ga_s[:, dch, :], op=ALU.mult),
                     r=[r_bank[ba], r_ga_s], w=[r_t1])
                P.op("dve", lambda e, bb=bb, dch=dch, t2=t2: e.tensor_tensor(out=t2, in0=banks[bb], in1=gb_s[:, dch, :], op=ALU.mult),
                     r=[r_bank[bb], r_gb_s], w=[r_t2])
                P.op("pool", lambda e, dch=dch, t1=t1, t2=t2: e.tensor_tensor(out=yT[:, dch, :], in0=t1, in1=t2, op=ALU.add),
                     r=[r_t1, r_t2], w=[r_yT])
            for t in range(4):
                xt, r_xt = fxt[t % 2]; x1t, r_x1t = fx1[t % 2]
                P.dma("sp", xt, xq_v[s * 4 + t], w=[r_xt])
                for half in range(2):
                    b = nbank()
                    for dch in range(8):
                        P.op("pe", lambda e, b=b, dch=dch, t=t, half=half: e.matmul(
                            banks[b], lhsT=yT[:, dch, t * 128:(t + 1) * 128], rhs=Wout[:, dch, half * 512:(half + 1) * 512],
                            start=(dch == 0), stop=(dch == 7)), r=[r_yT, r_Wout], w=[r_bank[b]])
                    P.op("dve", lambda e, b=b, half=half, xt=xt, x1t=x1t: e.tensor_tensor(
                        out=x1t[:, half * 512:(half + 1) * 512], in0=banks[b], in1=xt[:, half * 512:(half + 1) * 512],
                        op=ALU.add), r=[r_bank[b], r_xt], w=[r_x1t])
                P.dma("sp", x1_d[s * 512 + t * 128:s * 512 + (t + 1) * 128, :], x1t, r=[r_x1t], w=[r_x1_d])
            M.pop()
            P.barrier()
            rr["pool"] = [0, 1, 2, 3, 4, 5]
            return False

        for s_ in range(nslots):
            if do_slot(s_):
                return nc
        M.pop()
        P.barrier()
        rr["pool"] = list(range(8))
        if upto in ("F", "F1"):
            P.emit()
            return nc

        M.push()
        gffn_b = M.alloc(F32, [D]); r_gffn = Reg("gffn")
        gfin_b = M.alloc(F32, [D]); r_gfin = Reg("gfin")
        P.dma("sp", gffn_b, g_ffn.partition_broadcast(128), w=[r_gffn])
        P.dma("sp", gfin_b, g_fin.partition_broadcast(128), w=[r_gfin])
        Wr = M.alloc(F32, [8, 40]); r_Wr = Reg("Wr")
        for c in range(8):
            P.dma("sp", Wr[:, c, 0:8], w_rg[c * 128:(c + 1) * 128, :], w=[r_Wr])
            P.dma("sp", Wr[:, c, 8:40], w_re[c * 128:(c + 1) * 128, :], w=[r_Wr])
        brb = M.alloc(F32, [40]); r_brb = Reg("brb")
        P.dma("sp", brb[:, 0:8], b_rg.partition_broadcast(128), w=[r_brb])
        P.dma("sp", brb[:, 8:40], b_re.partition_broadcast(128), w=[r_brb])
        h2T = M.alloc(BF16, [8, NQ]); r_h2T = [Reg(f"h2T{i}") for i in range(4)]
        acc = M.alloc(F32, [16, D]); r_acc = [[Reg(f"acc{i}_{h}") for h in range(2)] for i in range(16)]
        comb = M.alloc(F32, [16, 32]); r_comb = [Reg(f"comb{i}") for i in range(16)]
        if debug:
            P.op("pool", lambda e: e.memset(comb, 0.0), w=r_comb)
        mx = []
        for i in range(2):
            mx.append(dict(xt=M.alloc(F32, [D]), r_xt=Reg(f"mxt{i}"), xn=M.alloc(BF16, [D]), r_xn=Reg(f"mxn{i}"),
                           xf=M.alloc(F32, [D]), r_xf=Reg(f"mxf{i}"), st=M.alloc(F32, [4]), r_st=Reg(f"mst{i}"),
                           jk=M.alloc(BF16, [D]), r_jk=Reg(f"mjk{i}"), hf=M.alloc(F32, [8, 128]), r_hf=Reg(f"mhf{i}"),
                           lg=M.alloc(F32, [40]), msk=M.alloc(F32, [32]), sm=M.alloc(F32, [48]), r_sm=Reg(f"msm{i}")))
        x1_v = x1_d.rearrange("(n p) d -> n p d", p=128)
        ntile = 16 if not _os.environ.get("KMOET") else int(_os.environ["KMOET"])
        nexp = 32 if not _os.environ.get("KMOEE") else int(_os.environ["KMOEE"])
        if _os.environ.get("KMOEONLY"):
            for t in range(16):
                P.dma("sp", x1_v[t], xq_v[t], w=[r_x1_d])
        def moe_stageA(t):
            m = mx[t % 2]
            xt, r_xt, xn, r_xn, xf, r_xf, st, r_st, jk, r_jk = (m["xt"], m["r_xt"], m["xn"], m["r_xn"], m["xf"], m["r_xf"],
                                                              m["st"], m["r_st"], m["jk"], m["r_jk"])
            hf, r_hf, lg, msk, sm, r_sm = m["hf"], m["r_hf"], m["lg"], m["msk"], m["sm"], m["r_sm"]
            P.dma("sp", xt, x1_v[t], r=[r_x1_d], w=[r_xt])
            P.op("act", lambda e, jk=jk, xt=xt, st=st: e.activation(out=jk, in_=xt, func=AF.Square, accum_out=st[:, 0:1]),
                 r=[r_xt], w=[r_jk, r_st])
            P.op("act", lambda e, st=st: e.activation(out=st[:, 1:2], in_=st[:, 0:1], func=AF.Sqrt, scale=1.0 / D, bias=EPS),
                 r=[r_st], w=[r_st])
            P.op("dve", lambda e, st=st: e.reciprocal(out=st[:, 2:3], in_=st[:, 1:2]), r=[r_st], w=[r_st])
            P.op("dve", lambda e, xf=xf, xt=xt, st=st: e.scalar_tensor_tensor(
                out=xf, in0=xt, scalar=st[:, 2:3], in1=gffn_b, op0=ALU.mult, op1=ALU.mult),
                r=[r_xt, r_st, r_gffn], w=[r_xf])
            P.op("pool", lambda e, xn=xn, xf=xf: e.tensor_copy(out=xn, in_=xf), r=[r_xf], w=[r_xn])
            b = nbank()
            tp = banks[b].bitcast(BF16)
            for c in range(8):
                P.op("pe", lambda e, tp=tp, xn=xn, c=c: e.transpose(
                    out=tp[:, c * 128:(c + 1) * 128], in_=xn[:, c * 128:(c + 1) * 128], identity=ident),
                    r=[r_xn, r_ident], w=[r_bank[b]])
            evac("act", h2T[:, :, t * 128:(t + 1) * 128], tp.rearrange("p (c k) -> p c k", c=8), [r_bank[b]],
                 [r_h2T[t // 4]])
            for half in range(2):
                b = nbank()
                for c4 in range(4):
                    c = half * 4 + c4
                    P.op("pe", lambda e, b=b, c=c, c4=c4, xf=xf: e.transpose(
                        out=banks[b][:, c4 * 128:(c4 + 1) * 128], in_=xf[:, c * 128:(c + 1) * 128], identity=ident_f),
                        r=[r_xf, r_identf], w=[r_bank[b]])
                evac("dve", hf[:, half * 4:(half + 1) * 4, :], banks[b].rearrange("p (c k) -> p c k", c=4),
                     [r_bank[b]], [r_hf])
            b = nbank()
            for c in range(8):
                P.op("pe", lambda e, b=b, c=c, hf=hf: e.matmul(banks[b][:, 0:40], lhsT=hf[:, c, :], rhs=Wr[:, c, :],
                                                             start=(c == 0), stop=(c == 7)),
                     r=[r_hf, r_Wr], w=[r_bank[b]])
            return b

        def moe_stageB(t, b):
            m = mx[t % 2]
            lg, msk, sm, r_sm = m["lg"], m["msk"], m["sm"], m["r_sm"]
            rsm = [r_sm]
            P.op("dve", lambda e, b=b, lg=lg: e.tensor_tensor(out=lg, in0=banks[b][:, 0:40], in1=brb, op=ALU.add),
                 r=[r_bank[b], r_brb], w=rsm)
            P.op("dve", lambda e, lg=lg, sm=sm: e.tensor_reduce(out=sm[:, 0:1], in_=lg[:, 0:8], axis=AX.X, op=ALU.max),
                 r=rsm, w=rsm)
            P.op("dve", lambda e, sm=sm: e.tensor_scalar(out=sm[:, 1:2], in0=sm[:, 0:1], scalar1=-1.0, scalar2=None,
                                                         op0=ALU.mult), r=rsm, w=rsm)
            P.op("act", lambda e, lg=lg, sm=sm: e.activation(out=sm[:, 8:16], in_=lg[:, 0:8], func=AF.Exp, bias=sm[:, 1:2],
                                                            scale=1.0, accum_out=sm[:, 2:3]), r=rsm, w=rsm)
            P.op("dve", lambda e, sm=sm: e.reciprocal(out=sm[:, 3:4], in_=sm[:, 2:3]), r=rsm, w=rsm)
            P.op("dve", lambda e, lg=lg, sm=sm: e.tensor_scalar(out=sm[:, 16:24], in0=lg[:, 0:8], scalar1=sm[:, 0:1],
                                                               scalar2=None, op0=ALU.is_equal), r=rsm, w=rsm)
            P.op("dve", lambda e, sm=sm: e.tensor_scalar(out=sm[:, 24:32], in0=sm[:, 16:24], scalar1=-1.0, scalar2=1.0e30,
                                                         op0=ALU.add, op1=ALU.mult), r=rsm, w=rsm)
            P.op("dve", lambda e, lg=lg, sm=sm, msk=msk: e.tensor_tensor(
                out=msk.rearrange("p (g k) -> p g k", k=4), in0=lg[:, 8:40].rearrange("p (g k) -> p g k", k=4),
                in1=sm[:, 24:32].rearrange("p (g o) -> p g o", o=1).to_broadcast([128, 8, 4]), op=ALU.add),
                r=rsm, w=rsm)
            P.op("dve", lambda e, sm=sm, msk=msk: e.max(out=sm[:, 32:40], in_=msk), r=rsm, w=rsm)
            P.op("dve", lambda e, sm=sm: e.tensor_scalar(out=sm[:, 4:5], in0=sm[:, 32:33], scalar1=-1.0, scalar2=None,
                                                         op0=ALU.mult), r=rsm, w=rsm)
            P.op("act", lambda e, sm=sm: e.activation(out=sm[:, 5:6], in_=sm[:, 33:34], func=AF.Exp, bias=sm[:, 4:5],
                                                     scale=1.0), r=rsm, w=rsm)
            P.op("dve", lambda e, sm=sm: e.tensor_scalar(out=sm[:, 6:7], in0=sm[:, 5:6], scalar1=1.0, scalar2=None,
                                                         op0=ALU.add), r=rsm, w=rsm)
            P.op("dve", lambda e, sm=sm: e.reciprocal(out=sm[:, 6:7], in_=sm[:, 6:7]), r=rsm, w=rsm)
            P.op("dve", lambda e, sm=sm: e.tensor_tensor(out=sm[:, 6:7], in0=sm[:, 6:7], in1=sm[:, 3:4], op=ALU.mult),
                 r=rsm, w=rsm)
            P.op("dve", lambda e, sm=sm: e.tensor_tensor(out=sm[:, 7:8], in0=sm[:, 6:7], in1=sm[:, 5:6], op=ALU.mult),
                 r=rsm, w=rsm)
            P.op("dve", lambda e, sm=sm, msk=msk, lg=lg: e.tensor_scalar(
                out=lg[:, 8:40], in0=msk, scalar1=sm[:, 32:33], scalar2=sm[:, 6:7], op0=ALU.is_equal, op1=ALU.mult),
                r=rsm, w=rsm)
            P.op("dve", lambda e, sm=sm, msk=msk: e.tensor_scalar(
                out=msk, in0=msk, scalar1=sm[:, 33:34], scalar2=sm[:, 7:8], op0=ALU.is_equal, op1=ALU.mult),
                r=rsm, w=rsm)
            P.op("dve", lambda e, t=t, msk=msk, lg=lg: e.tensor_tensor(out=comb[:, t, :], in0=lg[:, 8:40], in1=msk, op=ALU.add),
                 r=rsm, w=[r_comb[t]])

        prevb = None
        for t in range(ntile):
            bb_ = moe_stageA(t)
            if prevb is not None:
                moe_stageB(t - 1, prevb)
            prevb = bb_
        if prevb is not None:
            moe_stageB(ntile - 1, prevb)
        if debug:
            P.dma("sp", comb_d, comb.rearrange("p a n -> p (a n)"), r=r_comb, w=[r_dbg])
        wgs = [(M.alloc(BF16, [8, 256]), Reg(f"wg{i}")) for i in range(2)]
        wus = [(M.alloc(BF16, [8, 256]), Reg(f"wu{i}")) for i in range(2)]
        wds = [(M.alloc(BF16, [2, D]), Reg(f"wd{i}")) for i in range(2)]
        wdf = [(M.alloc(F32, [2, D]), Reg(f"wdf{i}")) for i in range(2)]
        sgs = [(M.alloc(F32, [512]), Reg(f"sg{i}")) for i in range(2)]
        hids = [(M.alloc(BF16, [2, 512]), Reg(f"hid{i}")) for i in range(2)]
        tms = [(M.alloc(F32, [512]), Reg(f"tm{i}")) for i in range(2)]
        k = 0
        for ex in range(nexp):
            Wg_e, r_Wg = wgs[ex % 2]; Wu_e, r_Wu = wus[ex % 2]; Wd_e, r_Wd = wds[ex % 2]
            if ex < 2 or not _os.environ.get("KMOENODMA"):
                P.dma("pool", Wg_e, w_gate[ex].rearrange("(c p) f -> p c f", p=128), w=[r_Wg])
                P.dma("pool", Wu_e, w_up[ex].rearrange("(c p) f -> p c f", p=128), w=[r_Wu])
                Wdf_e, r_Wdf = wdf[ex % 2]
                P.dma("sp", Wdf_e, w_down[ex].rearrange("(c p) n -> p c n", p=128), w=[r_Wdf])
                P.op("pool", lambda e, Wd_e=Wd_e, Wdf_e=Wdf_e: e.tensor_copy(out=Wd_e, in_=Wdf_e), r=[r_Wdf], w=[r_Wd])
            for blk in range((ntile + 3) // 4):
                hid, r_hid = hids[k % 2]; k += 1
                for ffc in range(2):
                    bg = nbank()
                    for c in range(8):
                        P.op("pe", lambda e, bg=bg, c=c, ffc=ffc, blk=blk, Wg_e=Wg_e: e.matmul(
                            banks[bg], lhsT=Wg_e[:, c, ffc * 128:(ffc + 1) * 128], rhs=h2T[:, c, blk * 512:(blk + 1) * 512],
                            start=(c == 0), stop=(c == 7)), r=[r_Wg, r_h2T[blk]], w=[r_bank[bg]])
                    bu = nbank()
                    for c in range(8):
                        P.op("pe", lambda e, bu=bu, c=c, ffc=ffc, blk=blk, Wu_e=Wu_e: e.matmul(
                            banks[bu], lhsT=Wu_e[:, c, ffc * 128:(ffc + 1) * 128], rhs=h2T[:, c, blk * 512:(blk + 1) * 512],
                            start=(c == 0), stop=(c == 7)), r=[r_Wu, r_h2T[blk]], w=[r_bank[bu]])
                    sg, r_sg = sgs[ffc]
                    P.op("act", lambda e, bg=bg, sg=sg: e.activation(out=sg, in_=banks[bg], func=AF.Silu),
                         r=[r_bank[bg]], w=[r_sg])
                    P.op("dve", lambda e, bu=bu, sg=sg, hid=hid, ffc=ffc: e.tensor_tensor(
                        out=hid[:, ffc, :], in0=banks[bu], in1=sg, op=ALU.mult), r=[r_bank[bu], r_sg], w=[r_hid])
                for t4 in range(4):
                    t = blk * 4 + t4
                    if t >= ntile:
                        continue
                    for half in range(2):
                        bd = nbank()
                        for ffc in range(2):
                            P.op("pe", lambda e, bd=bd, ffc=ffc, t4=t4, half=half, hid=hid, Wd_e=Wd_e: e.matmul(
                                banks[bd], lhsT=hid[:, ffc, t4 * 128:(t4 + 1) * 128],
                                rhs=Wd_e[:, ffc, half * 512:(half + 1) * 512], start=(ffc == 0), stop=(ffc == 1)),
                                r=[r_hid, r_Wd], w=[r_bank[bd]])
                        a_ = acc[:, t, half * 512:(half + 1) * 512]
                        cw = comb[:, t, ex:ex + 1]
                        if ex == 0:
                            P.op("dve", lambda e, bd=bd, a_=a_, cw=cw: e.tensor_scalar(
                                out=a_, in0=banks[bd], scalar1=cw, scalar2=None, op0=ALU.mult),
                                r=[r_bank[bd], r_comb[t]], w=[r_acc[t][half]])
                        elif half == 0:
                            P.op("dve", lambda e, bd=bd, a_=a_, cw=cw: e.scalar_tensor_tensor(
                                out=a_, in0=banks[bd], scalar=cw, in1=a_, op0=ALU.mult, op1=ALU.add),
                                r=[r_bank[bd], r_comb[t], r_acc[t][half]], w=[r_acc[t][half]])
                        else:
                            tm, r_tm = tms[t4 % 2]
                            P.op("act", lambda e, bd=bd, tm=tm, cw=cw: e.activation(
                                out=tm, in_=banks[bd], func=AF.Copy, scale=cw), r=[r_bank[bd], r_comb[t]], w=[r_tm])
                            P.op("pool", lambda e, a_=a_, tm=tm: e.tensor_tensor(out=a_, in0=a_, in1=tm, op=ALU.add),
                                 r=[r_tm, r_acc[t][half]], w=[r_acc[t][half]])
        for t in range(ntile):
            m = mx[t % 2]
            xt, r_xt, xf, r_xf, st, r_st, jk, r_jk = m["xt"], m["r_xt"], m["xf"], m["r_xf"], m["st"], m["r_st"], m["jk"], m["r_jk"]
            P.dma("sp", xt, x1_v[t], r=[r_x1_d], w=[r_xt])
            P.op("pool", lambda e, xt=xt, t=t: e.tensor_tensor(out=xt, in0=xt, in1=acc[:, t, :], op=ALU.add),
                 r=[r_xt] + r_acc[t], w=[r_xt])
            P.op("act", lambda e, jk=jk, xt=xt, st=st: e.activation(out=jk, in_=xt, func=AF.Square, accum_out=st[:, 0:1]),
                 r=[r_xt], w=[r_jk, r_st])
            P.op("act", lambda e, st=st: e.activation(out=st[:, 1:2], in_=st[:, 0:1], func=AF.Sqrt, scale=1.0 / D, bias=EPS),
                 r=[r_st], w=[r_st])
            P.op("dve", lambda e, st=st: e.reciprocal(out=st[:, 2:3], in_=st[:, 1:2]), r=[r_st], w=[r_st])
            P.op("dve", lambda e, xf=xf, xt=xt, st=st: e.scalar_tensor_tensor(
                out=xf, in0=xt, scalar=st[:, 2:3], in1=gfin_b, op0=ALU.mult, op1=ALU.mult),
                r=[r_xt, r_st, r_gfin], w=[r_xf])
            P.dma("sp", out[t * 128:(t + 1) * 128, :], xf, r=[r_xf])
        M.pop()

        P.emit()
    return nc


def perm_matrix(block, half):
    pm = np.zeros((128, 128), np.float32)
    for m in range(128):
        j = m % block
        if j < half:
            pm[m + half, m] = 1.0
        elif j < 2 * half:
            pm[m - half, m] = 1.0
    return pm


def nc_input_names(nc):
    return list(getattr(nc, "_in_names"))


def make_in_maps(inputs):
    x = np.ascontiguousarray(inputs["x"], dtype=np.float32)
    cs16, cs32 = rope_tables()
    shared = {
        "w_in": inputs["w_in"][0], "w_uq": inputs["w_uq"][0], "w_uk": inputs["w_uk"][0],
        "w_uv": inputs["w_uv"][0], "w_o_a": inputs["w_o_a"][0], "w_o_b": inputs["w_o_b"][0],
        "w_out": inputs["w_out"][0], "w_rg": inputs["w_router_group"][0], "w_re": inputs["w_router_expert"][0],
        "b_rg": inputs["b_router_group"][0], "b_re": inputs["b_router_expert"][0],
        "w_gate": inputs["w_gate"][0], "w_up": inputs["w_up"][0], "w_down": inputs["w_down"][0],
        "g_mix": inputs["norm_mix_g"][0], "g_q": inputs["mla_q_norm_g"][0], "g_kv": inputs["mla_kv_norm_g"][0],
        "g_ffn": inputs["norm_ffn_g"][0], "g_fin": inputs["final_norm_g"],
        "cs16k": cs16, "cs32k": cs32, "pm16": perm_matrix(64, 8), "pm32": perm_matrix(32, 16),
    }
    shared = {k: np.ascontiguousarray(v, dtype=np.float32) for k, v in shared.items()}
    in_maps = []
    for c in range(8):
        b, r = divmod(c, 4)
        rows = np.concatenate([np.arange((4 * s + r) * 512, (4 * s + r + 1) * 512) for s in range(4)])
        m = dict(shared)
        m["xb"] = x[b]
        m["xq"] = np.ascontiguousarray(x[b][rows])
        m["cs16q"] = np.ascontiguousarray(cs16[:, :, rows])
        m["cs32q"] = np.ascontiguousarray(cs32[:, :, rows])
        m["cs32q4"] = np.ascontiguousarray(np.tile(cs32[:, :, rows], (1, 4, 1)))
        qpos = 512 * r + np.arange(512)
        kpos = np.arange(2048)
        adm = (kpos[None, :] // 64) <= (qpos[:, None] // 64)
        m["cmask"] = np.where(adm, 0.0, NEGM).astype(np.float32).reshape(4, 128, 2048)
        m["cmaskT"] = np.ascontiguousarray(
            adm.T.astype(np.float32).reshape(16, 128, 512).transpose(1, 0, 2))
        in_maps.append(m)
    return in_maps


def kernel(**inputs):
    nc = build()
    in_maps = make_in_maps(inputs)
    names = set(nc_input_names(nc))
    in_maps = [{k: v for k, v in m.items() if k in names} for m in in_maps]
    res = run_bass_kernel_spmd(nc, in_maps, core_ids=list(range(8)))
    outp = np.zeros((2, S, D), np.float32)
    for c in range(8):
        b, r = divmod(c, 4)
        o = res.results[c]["out"]
        for s in range(4):
            g = 4 * s + r
            outp[b, g * 512:(g + 1) * 512] = o[s * 512:(s + 1) * 512]
    return outp
```
